# Optimizing a Trainium2 kernel written in Bass

```python
import math, functools
import jax, jax.numpy as jnp
from jax import lax
import numpy as np

D_MODEL = 1024
BATCH = 8
SEQ = 4096
DEPTH = 4
DEC_BATCH = 8
DEC_SEQ = 2048
PAST_LEN = 128

S5_WIDTH = D_MODEL // 4
S5_GROUP = 16
S5_GROUPS = S5_WIDTH // S5_GROUP
S5_STATE = 64
CONV_WIDTH = D_MODEL // 4
GDN_WIDTH = D_MODEL // 2
GDN_HEADS = 4
GDN_HEAD_DIM = GDN_WIDTH // GDN_HEADS
GDN_CHUNK = 64
SHORT_CONV = 3
MIX_WIDTH = S5_WIDTH + CONV_WIDTH + GDN_WIDTH
IN_COLS = S5_WIDTH + 3 * CONV_WIDTH + 4 * GDN_WIDTH + 4 * GDN_HEADS
MOE_GROUPS = 4
EXPERTS_PER_GROUP = 8
N_EXPERTS = MOE_GROUPS * EXPERTS_PER_GROUP
TOP_K = 2
EXPERT_FF = 512
MOE_BLOCK = 512
EPS = 1e-6

kernel_name = 'hybrid_bidir_s5_conv_gdn_hmoe_encoder'


def rms_norm(x, w):
    xf = x.astype(jnp.float32)
    xf = xf * lax.rsqrt(jnp.mean(xf * xf, axis=-1, keepdims=True) + EPS)
    return xf.astype(x.dtype) * w


def conv3_centred(x, w):
    xp = jnp.pad(x, ((0, 0), (1, 1), (0, 0)))
    return xp[:, :-2] * w[0] + xp[:, 1:-1] * w[1] + xp[:, 2:] * w[2]


def _in_split_points():
    widths = [S5_WIDTH, CONV_WIDTH, CONV_WIDTH, CONV_WIDTH, 3 * GDN_WIDTH, GDN_WIDTH,
              GDN_HEADS, GDN_HEADS, GDN_HEADS]
    points, acc = [], 0
    for wd in widths:
        acc += wd
        points.append(acc)
    return points


def _cmul(ar, ai, br, bi):
    return ar * br - ai * bi, ar * bi + ai * br


def _ssm_combine(e1, e2):
    a1r, a1i, b1r, b1i = e1
    a2r, a2i, b2r, b2i = e2
    ar, ai = _cmul(a2r, a2i, a1r, a1i)
    br, bi = _cmul(a2r, a2i, b1r, b1i)
    return ar, ai, br + b2r, bi + b2i


def s5_mixer(u, lam_re, lam_im, log_step, b_re, b_im, c_re, c_im, d_skip, w_glu, b_glu):
    bsz, seq, _ = u.shape
    uf = u.astype(jnp.float32)
    ug = uf.reshape(bsz, seq, S5_GROUPS, S5_GROUP)
    y = uf * d_skip.astype(jnp.float32)
    for direction in range(2):
        lr = lam_re[direction].astype(jnp.float32)
        li = lam_im[direction].astype(jnp.float32)
        dt = jnp.exp(log_step[direction].astype(jnp.float32))[:, None]
        mag = jnp.exp(lr * dt)
        abr, abi = mag * jnp.cos(li * dt), mag * jnp.sin(li * dt)
        den = lr * lr + li * li
        cr = ((abr - 1.0) * lr + abi * li) / den
        ci = (abi * lr - (abr - 1.0) * li) / den
        bbr, bbi = _cmul(cr[..., None], ci[..., None],
                         b_re[direction].astype(jnp.float32), b_im[direction].astype(jnp.float32))
        xr = jnp.einsum('blgh,gph->blgp', ug, bbr)
        xi = jnp.einsum('blgh,gph->blgp', ug, bbi)
        ar = jnp.broadcast_to(abr, xr.shape)
        ai = jnp.broadcast_to(abi, xr.shape)
        _, _, hr, hi = lax.associative_scan(_ssm_combine, (ar, ai, xr, xi),
                                            reverse=(direction == 1), axis=1)
        yg = (jnp.einsum('blgp,ghp->blgh', hr, c_re[direction].astype(jnp.float32))
              - jnp.einsum('blgp,ghp->blgh', hi, c_im[direction].astype(jnp.float32)))
        y = y + yg.reshape(bsz, seq, S5_WIDTH)
    y = jax.nn.gelu(y)
    out = y * jax.nn.sigmoid(y @ w_glu.astype(jnp.float32) + b_glu.astype(jnp.float32))
    return out.astype(u.dtype)


def _l2norm(x):
    return x * lax.rsqrt(jnp.sum(x * x, axis=-1, keepdims=True) + EPS)


def gdn_chunked(q, k, v, g, beta):
    bsz, nh, seq, dk = q.shape
    dv = v.shape[-1]
    nc = seq // GDN_CHUNK
    chunk = lambda t: t.reshape(bsz, nh, nc, GDN_CHUNK, *t.shape[3:])
    q, k, v, g, beta = chunk(q), chunk(k), chunk(v), chunk(g), chunk(beta)
    g = jnp.cumsum(g, axis=-1)
    idx = jnp.arange(GDN_CHUNK)
    incl = idx[:, None] >= idx[None, :]
    strict = idx[:, None] > idx[None, :]
    decay = jnp.exp(jnp.where(incl, g[..., :, None] - g[..., None, :], -jnp.inf))
    k_beta = k * beta[..., None]
    lower = jnp.where(strict, jnp.einsum('bhncd,bhnsd->bhncs', k_beta, k) * decay, 0.0)
    solve = functools.partial(lax.linalg.triangular_solve, left_side=True, lower=True,
                              unit_diagonal=True)
    u = solve(lower, v * beta[..., None])
    w = solve(lower, k_beta * jnp.exp(g)[..., None])
    intra = jnp.einsum('bhncd,bhnsd->bhncs', q, k) * decay
    g_last = g[..., -1]
    k_end = k * jnp.exp(g_last[..., None] - g)[..., None]
    q_dec = q * jnp.exp(g)[..., None]

    def step(state, inp):
        qc, kc, uc, wc, ac, gl = inp
        v_new = uc - jnp.einsum('bhcd,bhde->bhce', wc, state)
        out = jnp.einsum('bhcd,bhde->bhce', qc, state) + jnp.einsum('bhcs,bhse->bhce', ac, v_new)
        state = state * jnp.exp(gl)[..., None, None] + jnp.einsum('bhcd,bhce->bhde', kc, v_new)
        return state, out

    xs = tuple(jnp.moveaxis(t, 2, 0) for t in (q_dec, k_end, u, w, intra, g_last))
    state0 = jnp.zeros((bsz, nh, dk, dv), jnp.float32)
    _, out = lax.scan(step, state0, xs)
    return jnp.moveaxis(out, 0, 2).reshape(bsz, nh, seq, dv)


def gdn_mixer(qkv, z, a_f, a_b, b_f, b_b, conv_w, a_log, dt_bias, norm_w):
    bsz, seq, _ = qkv.shape
    out_dtype = qkv.dtype
    qkv = jax.nn.silu(conv3_centred(qkv, conv_w)).astype(jnp.float32)
    q, k, v = jnp.split(qkv, 3, axis=-1)
    heads = lambda t: t.reshape(bsz, seq, GDN_HEADS, GDN_HEAD_DIM).transpose(0, 2, 1, 3)
    q = _l2norm(heads(q)) * (GDN_HEAD_DIM ** -0.5)
    k = _l2norm(heads(k))
    v = heads(v)

    def gates(a_in, b_in, direction):
        g = -jnp.exp(a_log[direction].astype(jnp.float32)) * jax.nn.softplus(
            a_in.astype(jnp.float32) + dt_bias[direction].astype(jnp.float32))
        beta = jax.nn.sigmoid(b_in.astype(jnp.float32))
        return g.transpose(0, 2, 1), beta.transpose(0, 2, 1)

    g_fw, beta_fw = gates(a_f, b_f, 0)
    g_bw, beta_bw = gates(a_b, b_b, 1)
    flip = lambda t: jnp.flip(t, axis=2)
    o_fw = gdn_chunked(q, k, v, g_fw, beta_fw)
    o_bw = flip(gdn_chunked(flip(q), flip(k), flip(v), flip(g_bw), flip(beta_bw)))
    o = (o_fw + o_bw).transpose(0, 2, 1, 3)
    zh = z.astype(jnp.float32).reshape(bsz, seq, GDN_HEADS, GDN_HEAD_DIM)
    o = rms_norm(o, norm_w.astype(jnp.float32)) * jax.nn.silu(zh)
    return o.reshape(bsz, seq, GDN_WIDTH).astype(out_dtype)


def hier_moe(h, w_group, b_group, w_expert, b_expert, w_gate_up, w_down):
    bsz, seq, d = h.shape
    n_tok = bsz * seq
    n_rows = n_tok * TOP_K
    hf = h.reshape(n_tok, d)
    gprob = jax.nn.softmax((hf @ w_group + b_group).astype(jnp.float32), axis=-1)
    g_w, g_idx = lax.top_k(gprob, 1)
    elog = (hf @ w_expert + b_expert).astype(jnp.float32).reshape(n_tok, MOE_GROUPS, EXPERTS_PER_GROUP)
    elog = jnp.take_along_axis(elog, g_idx[:, :, None], axis=1)[:, 0]
    e_w, e_i = lax.top_k(jax.nn.softmax(elog, axis=-1), TOP_K)
    e_w = e_w / jnp.sum(e_w, axis=-1, keepdims=True) * g_w
    expert = (g_idx * EXPERTS_PER_GROUP + e_i).reshape(n_rows)
    weight = e_w.reshape(n_rows)
    token = jnp.arange(n_rows, dtype=jnp.int32) // TOP_K
    order = jnp.argsort(expert)
    e_sorted = expert[order]
    tok_sorted = token[order]
    counts = jnp.bincount(expert, length=N_EXPERTS)
    padded = (counts + MOE_BLOCK - 1) // MOE_BLOCK * MOE_BLOCK
    pad_end = jnp.cumsum(padded)
    pad_start = pad_end - padded
    start = jnp.cumsum(counts) - counts
    dest = pad_start[e_sorted] + jnp.arange(n_rows, dtype=jnp.int32) - start[e_sorted]
    n_blocks = -(-n_rows // MOE_BLOCK) + N_EXPERTS
    xs = jnp.zeros((n_blocks * MOE_BLOCK, d), h.dtype).at[dest].set(hf[tok_sorted])
    blk_expert = jnp.minimum(
        jnp.searchsorted(pad_end, jnp.arange(n_blocks) * MOE_BLOCK, side='right'), N_EXPERTS - 1)

    def expert_block(args):
        xb, e = args
        gate, up = jnp.split(xb @ w_gate_up[e], 2, axis=-1)
        return (jax.nn.silu(gate) * up) @ w_down[e]

    yb = lax.map(expert_block, (xs.reshape(n_blocks, MOE_BLOCK, d), blk_expert))
    y_rows = yb.reshape(n_blocks * MOE_BLOCK, d)[dest] * weight[order][:, None].astype(h.dtype)
    y = jax.ops.segment_sum(y_rows, tok_sorted, num_segments=n_tok)
    return y.reshape(bsz, seq, d)


def _trunk(x, c, w_ada, b_ada, norm_mix, norm_ffn, w_in, w_out,
           s5_lam_re, s5_lam_im, s5_log_step, s5_b_re, s5_b_im, s5_c_re, s5_c_im,
           s5_d, s5_w_glu, s5_b_glu, conv_w, gdn_conv_w, gdn_a_log, gdn_dt_bias, gdn_norm_w,
           moe_w_group, moe_b_group, moe_w_expert, moe_b_expert, moe_w_gate_up, moe_w_down,
           final_norm):
    split_points = _in_split_points()
    for l in range(DEPTH):
        mod = jax.nn.silu(c) @ w_ada[l] + b_ada[l]
        sh1, sc1, g1, sh2, sc2, g2 = jnp.split(mod[:, None, :], 6, axis=-1)
        h = rms_norm(x, norm_mix[l]) * (1 + sc1) + sh1
        proj = h @ w_in[l]
        u_s5, cx, cb, cc, qkv, z, a_f, a_b, b_f, b_b = jnp.split(proj, split_points, axis=-1)
        y_s5 = s5_mixer(u_s5, s5_lam_re[l], s5_lam_im[l], s5_log_step[l], s5_b_re[l], s5_b_im[l],
                        s5_c_re[l], s5_c_im[l], s5_d[l], s5_w_glu[l], s5_b_glu[l])
        y_conv = cb * conv3_centred(cc * cx, conv_w[l])
        y_gdn = gdn_mixer(qkv, z, a_f, a_b, b_f, b_b, gdn_conv_w[l], gdn_a_log[l],
                          gdn_dt_bias[l], gdn_norm_w[l])
        x = x + g1 * (jnp.concatenate([y_s5, y_conv, y_gdn], axis=-1) @ w_out[l])
        h = rms_norm(x, norm_ffn[l]) * (1 + sc2) + sh2
        x = x + g2 * hier_moe(h, moe_w_group[l], moe_b_group[l], moe_w_expert[l], moe_b_expert[l],
                              moe_w_gate_up[l], moe_w_down[l])
    return rms_norm(x, final_norm)


def setup_inputs(seed: int = 0) -> dict:
    key = jax.random.key(seed)
    ks = jax.random.split(key, 40)
    f32 = jnp.float32

    def nrm(k, shape, scale):
        return jax.random.normal(k, shape, f32) * scale

    L, G, P, H = DEPTH, S5_GROUPS, S5_STATE, S5_GROUP
    s5_log_step = jax.random.uniform(ks[10], (L, 2, G), f32, math.log(1e-3), math.log(1e-1))
    gdn_dt = jnp.exp(jax.random.uniform(ks[21], (L, 2, GDN_HEADS), f32, math.log(1e-3), math.log(1e-1)))
    return {
        'x_prompt': nrm(ks[0], (BATCH, SEQ, D_MODEL), 1.0),
        'x_sample': nrm(ks[1], (DEC_BATCH, DEC_SEQ, D_MODEL), 1.0),
        'c_prompt': nrm(ks[2], (BATCH, D_MODEL), 1.0),
        'c_sample': nrm(ks[3], (DEC_BATCH, D_MODEL), 1.0),
        'w_ada': nrm(ks[4], (L, D_MODEL, 6 * D_MODEL), 0.5 * D_MODEL ** -0.5),
        'b_ada': nrm(ks[5], (L, 6 * D_MODEL), 0.02),
        'norm_mix': 1.0 + nrm(ks[6], (L, D_MODEL), 0.02),
        'norm_ffn': 1.0 + nrm(ks[7], (L, D_MODEL), 0.02),
        'w_in': nrm(ks[8], (L, D_MODEL, IN_COLS), D_MODEL ** -0.5),
        'w_out': nrm(ks[9], (L, MIX_WIDTH, D_MODEL), MIX_WIDTH ** -0.5),
        's5_lam_re': -0.5 + nrm(ks[11], (L, 2, G, P), 0.01),
        's5_lam_im': jnp.pi * jnp.arange(P, dtype=f32) + nrm(ks[12], (L, 2, G, P), 0.01),
        's5_log_step': s5_log_step,
        's5_b_re': nrm(ks[13], (L, 2, G, P, H), (2 * H) ** -0.5),
        's5_b_im': nrm(ks[14], (L, 2, G, P, H), (2 * H) ** -0.5),
        's5_c_re': nrm(ks[15], (L, 2, G, H, P), P ** -0.5),
        's5_c_im': nrm(ks[16], (L, 2, G, H, P), P ** -0.5),
        's5_d': nrm(ks[17], (L, S5_WIDTH), 1.0),
        's5_w_glu': nrm(ks[18], (L, S5_WIDTH, S5_WIDTH), S5_WIDTH ** -0.5),
        's5_b_glu': nrm(ks[19], (L, S5_WIDTH), 0.02),
        'conv_w': nrm(ks[20], (L, SHORT_CONV, CONV_WIDTH), SHORT_CONV ** -0.5),
        'gdn_conv_w': nrm(ks[22], (L, SHORT_CONV, 3 * GDN_WIDTH), SHORT_CONV ** -0.5),
        'gdn_a_log': jnp.log(jax.random.uniform(ks[23], (L, 2, GDN_HEADS), f32, 1.0, 16.0)),
        'gdn_dt_bias': gdn_dt + jnp.log(-jnp.expm1(-gdn_dt)),
        'gdn_norm_w': 1.0 + nrm(ks[24], (L, GDN_HEAD_DIM), 0.02),
        'moe_w_group': nrm(ks[25], (L, D_MODEL, MOE_GROUPS), D_MODEL ** -0.5),
        'moe_b_group': nrm(ks[26], (L, MOE_GROUPS), 0.01),
        'moe_w_expert': nrm(ks[27], (L, D_MODEL, N_EXPERTS), D_MODEL ** -0.5),
        'moe_b_expert': nrm(ks[28], (L, N_EXPERTS), 0.01),
        'moe_w_gate_up': nrm(ks[29], (L, N_EXPERTS, D_MODEL, 2 * EXPERT_FF), D_MODEL ** -0.5),
        'moe_w_down': nrm(ks[30], (L, N_EXPERTS, EXPERT_FF, D_MODEL), EXPERT_FF ** -0.5),
        'final_norm': 1.0 + nrm(ks[31], (D_MODEL,), 0.02),
    }


def reference(x_prompt, x_sample, c_prompt, c_sample, w_ada, b_ada, norm_mix, norm_ffn, w_in, w_out,
              s5_lam_re, s5_lam_im, s5_log_step, s5_b_re, s5_b_im, s5_c_re, s5_c_im,
              s5_d, s5_w_glu, s5_b_glu, conv_w, gdn_conv_w, gdn_a_log, gdn_dt_bias, gdn_norm_w,
              moe_w_group, moe_b_group, moe_w_expert, moe_b_expert, moe_w_gate_up, moe_w_down,
              final_norm):
    params = (w_ada, b_ada, norm_mix, norm_ffn, w_in, w_out,
              s5_lam_re, s5_lam_im, s5_log_step, s5_b_re, s5_b_im, s5_c_re, s5_c_im,
              s5_d, s5_w_glu, s5_b_glu, conv_w, gdn_conv_w, gdn_a_log, gdn_dt_bias, gdn_norm_w,
              moe_w_group, moe_b_group, moe_w_expert, moe_b_expert, moe_w_gate_up, moe_w_down,
              final_norm)
    y_prompt = _trunk(x_prompt, c_prompt, *params)
    y_sample = _trunk(x_sample, c_sample, *params)
    return (y_prompt, y_sample)
```

```python
import os
import numpy as np
import concourse.bass as bass
import concourse.mybir as mybir
from concourse.bass_utils import run_bass_kernel_spmd

F32 = mybir.dt.float32
BF16 = mybir.dt.bfloat16
I32 = mybir.dt.int32
U32 = mybir.dt.uint32
AF = mybir.ActivationFunctionType
OP = mybir.AluOpType
AX = mybir.AxisListType

D = 1024
KC = 8
IN_COLS = 3088
NCH = 25
EPS = 1e-6
NEXP = 32
FF = 512
BLK = 512


class Res:
    __slots__ = ("name", "w", "r")

    def __init__(self, name=""):
        self.name = name
        self.w = None
        self.r = {}


class Eng:
    def __init__(self, P, name, eng):
        self.P = P
        self.name = name
        self.eng = eng
        self.sem = P.nc.alloc_semaphore("s_" + name)
        self.key = "E_" + name
        P.sems[self.key] = self.sem
        self.count = 0
        self.seen = {}


class Prog:
    NDMA = 20

    def __init__(self, nc):
        self.nc = nc
        self.sems = {}
        self.pe = Eng(self, "pe", nc.tensor)
        self.act = Eng(self, "act", nc.scalar)
        self.dve = Eng(self, "dve", nc.vector)
        self.pool = Eng(self, "pool", nc.gpsimd)
        self.sp = Eng(self, "sp", nc.sync)
        self.engs = [self.pe, self.act, self.dve, self.pool, self.sp]
        self.dma_sems = {}
        self.dma_rr = {}
        self.dma_cnt = {}
        for e in (self.sp, self.act, self.pool):
            lst = []
            for i in range(self.NDMA):
                key = "D_%s_%d" % (e.name, i)
                self.sems[key] = nc.alloc_semaphore(key)
                self.dma_cnt[key] = 0
                lst.append(key)
            self.dma_sems[e.name] = lst
            self.dma_rr[e.name] = 0
        self.ninst = 0

    def _wait(self, e, key, val):
        if val <= 0:
            return
        if e.seen.get(key, 0) >= val:
            return
        e.eng.wait_ge(self.sems[key], val)
        e.seen[key] = val

    def _deps(self, e, r, w):
        need = {}
        for res in r:
            if res.w is not None:
                k, v = res.w
                if need.get(k, 0) < v:
                    need[k] = v
        for res in w:
            if res.w is not None:
                k, v = res.w
                if need.get(k, 0) < v:
                    need[k] = v
            for k, v in res.r.items():
                if need.get(k, 0) < v:
                    need[k] = v
        for k, v in need.items():
            self._wait(e, k, v)

    def _commit(self, ev, r, w):
        k, v = ev
        for res in r:
            res.r[k] = v
        for res in w:
            res.w = ev
            res.r = {}

    def op(self, e, fn, r=(), w=()):
        self._deps(e, r, w)
        inst = fn()
        e.count += 1
        inst.then_inc(e.sem, 1)
        self._commit((e.key, e.count), r, w)
        self.ninst += 1
        return inst

    def dma(self, e, out, in_, r=(), w=(), **kw):
        lst = self.dma_sems[e.name]
        key = lst[self.dma_rr[e.name] % self.NDMA]
        self.dma_rr[e.name] += 1
        self._wait(e, key, self.dma_cnt[key])
        self._deps(e, r, w)
        inst = e.eng.dma_start(out=out, in_=in_, **kw)
        self.dma_cnt[key] += 16
        inst.then_inc(self.sems[key], 16)
        self._commit((key, self.dma_cnt[key]), r, w)
        self.ninst += 1
        return inst

    def idma(self, out, out_off, in_, in_off, r=(), w=(), **kw):
        e = self.pool
        lst = self.dma_sems[e.name]
        key = lst[self.dma_rr[e.name] % self.NDMA]
        self.dma_rr[e.name] += 1
        self._wait(e, key, self.dma_cnt[key])
        self._deps(e, r, w)
        inst = e.eng.indirect_dma_start(out=out, out_offset=out_off, in_=in_, in_offset=in_off, **kw)
        self.dma_cnt[key] += 16
        inst.then_inc(self.sems[key], 16)
        self._commit((key, self.dma_cnt[key]), r, w)
        self.ninst += 1
        return inst

    def barrier(self):
        for e in self.engs:
            for o in self.engs:
                if o is not e:
                    self._wait(e, o.key, o.count)
            for key, cnt in self.dma_cnt.items():
                self._wait(e, key, cnt)

    def finish(self):
        self.barrier()


class Rot:
    def __init__(self, tiles):
        self.tiles = tiles
        self.res = [Res() for _ in tiles]
        self.i = 0

    def next(self):
        j = self.i % len(self.tiles)
        self.i += 1
        return self.tiles[j], self.res[j]


import math
from contextlib import ExitStack
TWO_PI = 2.0 * math.pi


def build(LP, LS, depth, flags=None):
    flags = flags or {}
    en_s5 = flags.get("s5", True)
    en_conv = flags.get("conv", True)
    en_gdn = flags.get("gdn", True)
    en_moe = flags.get("moe", True)
    nc = bass.Bass("TRN2", target_bir_lowering=False)
    P = Prog(nc)
    pe, act, dve, pool, sp = P.pe, P.act, P.dve, P.pool, P.sp
    LT = LP + LS
    NT = LT // 128
    seqs = [(0, LP), (LP, LS)]
    LMAX = max(LP, LS)
    NBT = LT // 512

    def din(name, shape, dt=F32):
        return nc.dram_tensor(name, list(shape), dt, kind="ExternalInput").ap()

    def dscr(name, shape, dt=F32):
        return nc.dram_tensor(name, list(shape), dt, kind="Internal").ap()

    x_in = din("x_in", [LT, D])
    cT_in = din("cT", [128, KC, 2])
    w_ada = din("w_ada", [depth, D, 6 * D])
    b_ada_row = din("b_ada_row", [depth, 6 * D])
    b_ada_fm = din("b_ada_fm", [128, depth, 6, KC])
    nmix_fm = din("nmix_fm", [128, depth, KC])
    nffn_row = din("nffn_row", [depth, D])
    w_in_c = din("w_in_c", [depth, NCH, 128, KC * 128])
    w_out = din("w_out", [depth, D, D])
    convw_fm = din("convw_fm", [128, depth, 2, 3])
    fin_row = din("fin_row", [1, D])
    s5_lam = din("s5_lam", [depth, 2, 2048])
    s5_ls = din("s5_ls", [depth, 32])
    s5_bblk = din("s5_bblk", [depth, 2, 128, 2048])
    s5_cst = din("s5_cst", [depth, 128, 2 * 16 * 128])
    s5_wglu = din("s5_wglu", [depth, 256, 256])
    s5_vec_fm = din("s5_vec_fm", [128, depth, 2, 2])
    gdn_cw_fm = din("gdn_cw_fm", [128, depth, 12, 3])
    gdn_gate = din("gdn_gate", [depth, 16])
    gdn_nw = din("gdn_nw", [depth, 128])
    w_z = din("w_z", [depth, D, 512])
    w_g = din("w_g", [depth, D, 16])
    moe_wr = din("moe_wr", [depth, D, 36])
    moe_rb = din("moe_rb", [depth, 36])
    moe_wgu = din("moe_wgu", [depth, NEXP, D, 2 * FF])
    moe_wd = din("moe_wd", [depth, NEXP, FF, D])
    h2T_d = dscr("h2T_d", [NBT, 128, KC, 512], BF16)
    R_h2d = [Res() for _ in range(NBT)]
    ofw_d = dscr("ofw_d", [NT, 128, 512])
    R_ofw = [Res() for _ in range(NT)]
    y_out = nc.dram_tensor("y_out", [LT, D], F32, kind="ExternalOutput").ap()
    x_work = dscr("x_work", [LT, D])
    mod_row_d = dscr("mod_row_d", [2, depth, 6 * D])
    mixT_d = dscr("mixT_d", [NBT, 128, KC, 512], BF16)
    yacc_d = dscr("yacc_d", [NT, 128, 256])
    R_mixd = [[Res() for _ in range(NBT)] for _ in range(KC)]
    R_yacc = [Res() for _ in range(NT)]

    sb = lambda name, shape, dt=F32: nc.alloc_sbuf_tensor(name, list(shape), dt)
    ps = lambda name, shape, dt=F32: nc.alloc_psum_tensor(name, list(shape), dt)
    uid = [0]

    def scoped(es, name, shape, dt=F32):
        uid[0] += 1
        return es.enter_context(nc.sbuf_tensor("%s_%d" % (name, uid[0]), list(shape), dt))

    psF = Rot([ps("psF%d" % i, [128, 512]) for i in range(6)])
    psH = Rot([ps("psH%d" % i, [128, 1024], BF16) for i in range(2)])

    ident = sb("ident", [128, 128])
    ident_bf = sb("ident_bf", [128, 128], BF16)
    ones_f = sb("ones_f", [128, 128])
    triF = sb("triF", [128, 128], BF16)
    triB = sb("triB", [128, 128], BF16)
    selF = sb("selF", [128, 128])
    selB = sb("selB", [128, 128])
    nvec = sb("nvec", [128, 2])
    nneg = sb("nneg", [128, 2])
    nveci = sb("nveci", [128, 2], I32)
    R_const = Res("const")
    P.op(pool, lambda: nc.gpsimd.memset(ones_f[:], 1.0), w=[R_const])

    def asel(out, pattern, cmp, base, cm, in_=None):
        P.op(pool, lambda: nc.gpsimd.affine_select(out=out, in_=(ones_f[:] if in_ is None else in_), pattern=pattern,
                                                  compare_op=cmp, fill=0.0, base=base, channel_multiplier=cm),
             r=[R_const], w=[R_const])
    asel(ident[:], [[-1, 128]], OP.is_equal, 0, 1)
    asel(triF[:], [[1, 128]], OP.is_ge, 0, -1)
    asel(triB[:], [[-1, 128]], OP.is_ge, 0, 1)
    asel(selF[:], [[0, 128]], OP.is_equal, -127, 1)
    asel(selB[:], [[0, 128]], OP.is_equal, 0, 1)
    P.op(pool, lambda: nc.gpsimd.tensor_copy(out=ident_bf[:], in_=ident[:]), r=[R_const], w=[R_const])
    P.op(pool, lambda: nc.gpsimd.iota(nveci[:, 0:1], pattern=[[0, 1]], base=1, channel_multiplier=1), w=[R_const])
    P.op(pool, lambda: nc.gpsimd.iota(nveci[:, 1:2], pattern=[[0, 1]], base=128, channel_multiplier=-1), w=[R_const])
    P.op(pool, lambda: nc.gpsimd.tensor_copy(out=nvec[:], in_=nveci[:]), r=[R_const], w=[R_const])
    P.op(pool, lambda: nc.gpsimd.tensor_scalar(out=nneg[:], in0=nvec[:], scalar1=-1.0, scalar2=None, op0=OP.mult),
         r=[R_const], w=[R_const])

    triF32 = sb("triF32", [128, 128])
    triB32 = sb("triB32", [128, 128])
    asel(triF32[:], [[1, 128]], OP.is_ge, 0, -1)
    asel(triB32[:], [[-1, 128]], OP.is_ge, 0, 1)
    gML = sb("gML", [128, 2, 128])
    gMA = sb("gMA", [128, 2, 128])
    gBD = sb("gBD", [128, 128])
    asel(gML[:, 0, :], [[-1, 128]], OP.is_gt, 0, 1)
    asel(gML[:, 1, :], [[1, 128]], OP.is_gt, 0, -1)
    asel(gMA[:, 0, :], [[1, 128]], OP.is_ge, 0, -1)
    asel(gMA[:, 1, :], [[-1, 128]], OP.is_ge, 0, 1)
    P.op(pool, lambda: nc.gpsimd.memset(gBD[:], 0.0), w=[R_const])
    P.op(pool, lambda: nc.gpsimd.memset(gBD[0:64, 0:64], 1.0), w=[R_const])
    P.op(pool, lambda: nc.gpsimd.memset(gBD[64:128, 64:128], 1.0), w=[R_const])
    gcw = sb("gcw", [128, depth, 12, 3])
    P.dma(sp, gcw[:], gdn_cw_fm, w=[R_const])

    mod_fm = sb("mod_fm", [128, depth, 6, KC, 2])
    sc1s = sb("sc1s", [128, depth, KC, 2])
    cw = sb("cw", [128, depth, 2, 3])
    s5v = sb("s5v", [128, depth, 2, 2])
    finb = sb("finb", [128, D])
    R_mod = Res("mod")
    R_modrow = Res("modrow")
    with ExitStack() as es:
        cT = scoped(es, "cT_sb", [128, KC, 2])
        scT = scoped(es, "scT", [128, KC, 2])
        bfm = scoped(es, "bfm", [128, depth, 6, KC])
        brow = scoped(es, "brow", [2, 6 * D])
        mrow = scoped(es, "mrow", [2, 6 * D])
        nmix = scoped(es, "nmix", [128, depth, KC])
        wadas = [scoped(es, "wada%d" % i, [128, KC, D]) for i in range(2)]
        wada_rot = Rot(wadas)
        R_brow = Res()
        P.dma(sp, cT[:], cT_in, w=[R_mod])
        P.dma(sp, bfm[:], b_ada_fm, w=[R_mod])
        P.dma(sp, nmix[:], nmix_fm, w=[R_mod])
        P.dma(sp, cw[:], convw_fm, w=[R_const])
        P.dma(sp, s5v[:], s5_vec_fm, w=[R_const])
        P.dma(sp, finb[:], fin_row[0, :].partition_broadcast(128), w=[R_const])
        P.op(act, lambda: nc.scalar.activation(out=scT[:], in_=cT[:], func=AF.Silu), r=[R_mod], w=[R_mod])
        for l in range(depth):
            for j in range(2):
                P.dma(sp, brow[j:j + 1, :], b_ada_row[l:l + 1, :], w=[R_brow])
            for v in range(6):
                wt, wr = wada_rot.next()
                src_ = w_ada[l].rearrange("(kc p) f -> p kc f", p=128)[:, :, v * D:(v + 1) * D]
                P.dma(sp if (v % 2 == 0) else act, wt[:], src_, w=[wr])
                pt, pr = psF.next()

                def mm_fm():
                    ins = None
                    for fc in range(KC):
                        for kc in range(KC):
                            ins = nc.tensor.matmul(pt[:, fc * 2:fc * 2 + 2], lhsT=wt[:, kc, fc * 128:(fc + 1) * 128],
                                                   rhs=scT[:, kc, :], start=(kc == 0), stop=(kc == KC - 1))
                    return ins
                P.op(pe, mm_fm, r=[wr, R_mod], w=[pr])
                P.op(dve, lambda: nc.vector.tensor_tensor(
                    out=mod_fm[:, l, v, :, :], in0=pt[:, 0:16].rearrange("p (f j) -> p f j", j=2),
                    in1=bfm[:, l, v, :].unsqueeze(2).to_broadcast([128, KC, 2]), op=OP.add), r=[pr, R_mod], w=[R_mod])
                for half in range(2):
                    pt2, pr2 = psF.next()

                    def mm_row():
                        ins = None
                        for kc in range(KC):
                            ins = nc.tensor.matmul(pt2[0:2, :], lhsT=scT[:, kc, :],
                                                   rhs=wt[:, kc, half * 512:(half + 1) * 512],
                                                   start=(kc == 0), stop=(kc == KC - 1))
                        return ins
                    P.op(pe, mm_row, r=[wr, R_mod], w=[pr2])
                    c0 = v * D + half * 512
                    P.op(dve, lambda: nc.vector.tensor_tensor(out=mrow[:, c0:c0 + 512], in0=pt2[0:2, :],
                                                              in1=brow[:, c0:c0 + 512], op=OP.add),
                         r=[pr2, R_brow], w=[R_brow])
            P.dma(sp, mod_row_d[:, l, :], mrow[:, :], r=[R_brow], w=[R_modrow])
            P.op(dve, lambda: nc.vector.scalar_tensor_tensor(
                out=sc1s[:, l, :, :], in0=mod_fm[:, l, 1, :, :], scalar=1.0,
                in1=nmix[:, l, :].unsqueeze(2).to_broadcast([128, KC, 2]), op0=OP.add, op1=OP.mult),
                r=[R_mod], w=[R_mod])
        P.barrier()

    hT = sb("hT", [128, KC, LMAX], BF16)
    R_hT = [Res() for _ in range(LMAX // 128)]
    st_rot = Rot([sb("st%d" % i, [128, 4]) for i in range(4)])
    win_rot = Rot([sb("win%d" % i, [128, KC, 128], BF16) for i in range(4)])

    def rms_rstd(src_tile, src_res, junk_t, junk_r):
        st, sr = st_rot.next()
        P.op(act, lambda: nc.scalar.activation(out=junk_t, in_=src_tile, func=AF.Square, accum_out=st[:, 0:1]),
             r=[src_res], w=[junk_r, sr])
        P.op(dve, lambda: nc.vector.tensor_scalar(out=st[:, 1:2], in0=st[:, 0:1], scalar1=1.0 / D, scalar2=EPS,
                                                  op0=OP.mult, op1=OP.add), r=[sr], w=[sr])
        P.op(act, lambda: nc.scalar.activation(out=st[:, 2:3], in_=st[:, 1:2], func=AF.Sqrt), r=[sr], w=[sr])
        P.op(dve, lambda: nc.vector.reciprocal(out=st[:, 3:4], in_=st[:, 2:3]), r=[sr], w=[sr])
        return st, sr

    Wall = sb("Wall", [128, NT, NEXP])
    R_W = [Res() for _ in range(NT)]
    x_src = x_in
    for l in range(depth):
        def load_win(ch):
            wt, wr = win_rot.next()
            P.dma(pool, wt[:].rearrange("p k c -> p (k c)"), w_in_c[l, ch], w=[wr])
            return wt, wr

        def proj_block(wt, wr, b, M=128):
            pt, pr = psF.next()

            def mm():
                ins = None
                for kc in range(KC):
                    ins = nc.tensor.matmul(pt[0:M, :], lhsT=wt[:, kc, 0:M], rhs=hT[:, kc, b * 512:(b + 1) * 512],
                                           start=(kc == 0), stop=(kc == KC - 1))
                return ins
            P.op(pe, mm, r=[wr] + R_hT[b * 4:(b + 1) * 4], w=[pr])
            return pt, pr

        def s5_setup(s5es):
            tabs = scoped(s5es, "tabs", [128, 4, 2048])
            Bx = scoped(s5es, "Bx", [128, 2, 2, 8, 2, 64], BF16)
            Cst = scoped(s5es, "Cst", [128, 2, 16, 128], BF16)
            wglu = scoped(s5es, "wglu", [128, 2, 256], BF16)
            R_s5c = Res()
            P.dma(pool, wglu[:], s5_wglu[l].rearrange("(kc p) n -> p kc n", p=128), w=[R_s5c])
            with ExitStack() as es:
                lam = scoped(es, "lam", [128, 2, 1024])
                dtb = scoped(es, "dtb", [128, 16])
                bb = scoped(es, "bb", [128, 2, 1024])
                cstf = scoped(es, "cstf", [128, 2048])
                w = [scoped(es, "s5w%d" % i, [128, 1024]) for i in range(7)]
                wi = scoped(es, "s5wi", [128, 1024], I32)
                R_w = Res()
                V = lambda e, fn: P.op(e, fn, r=[R_w, R_const], w=[R_w])
                for d_ in range(2):
                    dc = slice(d_ * 1024, (d_ + 1) * 1024)
                    P.dma(sp, lam[:, 0, :], s5_lam[l, 0, dc].partition_broadcast(128), w=[R_w])
                    P.dma(sp, lam[:, 1, :], s5_lam[l, 1, dc].partition_broadcast(128), w=[R_w])
                    P.dma(sp, dtb[:], s5_ls[l, d_ * 16:(d_ + 1) * 16].partition_broadcast(128), w=[R_w])
                    P.dma(act, bb[:, 0, :], s5_bblk[l, 0][:, dc], w=[R_w])
                    P.dma(act, bb[:, 1, :], s5_bblk[l, 1][:, dc], w=[R_w])
                    P.dma(sp, cstf[:], s5_cst[l][:, d_ * 2048:(d_ + 1) * 2048], w=[R_w])
                    V(pool, lambda: nc.gpsimd.tensor_scalar(out=cstf[64:128, :], in0=cstf[64:128, :], scalar1=-1.0,
                                                           scalar2=None, op0=OP.mult))
                    P.op(pool, lambda: nc.gpsimd.tensor_copy(out=Cst[:, d_].rearrange("p g c -> p (g c)"), in_=cstf[:]),
                         r=[R_w], w=[R_s5c])
                    V(act, lambda: nc.scalar.activation(out=dtb[:], in_=dtb[:], func=AF.Exp))
                    dtbc = dtb[:].unsqueeze(2).to_broadcast([128, 16, 64])
                    v3 = lambda t: t[:].rearrange("p (a b) -> p a b", b=64)
                    lnmag, ang = w[0], w[1]
                    V(dve, lambda: nc.vector.tensor_tensor(out=v3(lnmag), in0=lam[:, 0, :].rearrange("p (a b) -> p a b", b=64),
                                                           in1=dtbc, op=OP.mult))
                    V(dve, lambda: nc.vector.tensor_tensor(out=v3(ang), in0=lam[:, 1, :].rearrange("p (a b) -> p a b", b=64),
                                                           in1=dtbc, op=OP.mult))

                    def sin_of(dst, src_ap_fn, shift, t1):
                        V(dve, lambda: nc.vector.tensor_scalar(out=t1[:], in0=src_ap_fn(), scalar1=shift,
                                                               scalar2=1.0 / TWO_PI, op0=OP.add, op1=OP.mult))
                        V(dve, lambda: nc.vector.tensor_copy(out=wi[:], in_=t1[:]))
                        V(dve, lambda: nc.vector.tensor_copy(out=dst[:], in_=wi[:]))
                        V(dve, lambda: nc.vector.tensor_tensor(out=t1[:], in0=t1[:], in1=dst[:], op=OP.subtract))
                        V(dve, lambda: nc.vector.tensor_scalar(out=dst[:], in0=t1[:], scalar1=0.5, scalar2=-1.0,
                                                               op0=OP.is_gt, op1=OP.mult))
                        V(dve, lambda: nc.vector.tensor_tensor(out=t1[:], in0=t1[:], in1=dst[:], op=OP.add))
                        V(dve, lambda: nc.vector.tensor_scalar(out=dst[:], in0=t1[:], scalar1=-0.5, scalar2=1.0,
                                                               op0=OP.is_lt, op1=OP.mult))
                        V(dve, lambda: nc.vector.tensor_tensor(out=t1[:], in0=t1[:], in1=dst[:], op=OP.add))
                        V(act, lambda: nc.scalar.activation(out=dst[:], in_=t1[:], func=AF.Sin, scale=TWO_PI))

                    mag, sn, cs, t1, t2 = w[2], w[3], w[4], w[5], w[6]
                    V(act, lambda: nc.scalar.activation(out=mag[:], in_=lnmag[:], func=AF.Exp))
                    sin_of(sn, lambda: ang[:], 0.0, t1)
                    sin_of(cs, lambda: ang[:], math.pi / 2, t1)
                    abr, abi = cs, sn
                    V(dve, lambda: nc.vector.tensor_tensor(out=abr[:], in0=cs[:], in1=mag[:], op=OP.mult))
                    V(dve, lambda: nc.vector.tensor_tensor(out=abi[:], in0=sn[:], in1=mag[:], op=OP.mult))
                    V(dve, lambda: nc.vector.tensor_scalar(out=abr[:], in0=abr[:], scalar1=-1.0, scalar2=None, op0=OP.add))
                    lr, li = lam[:, 0, :], lam[:, 1, :]
                    den = mag
                    V(dve, lambda: nc.vector.tensor_tensor(out=den[:], in0=lr, in1=lr, op=OP.mult))
                    V(dve, lambda: nc.vector.tensor_tensor(out=t1[:], in0=li, in1=li, op=OP.mult))
                    V(dve, lambda: nc.vector.tensor_tensor(out=den[:], in0=den[:], in1=t1[:], op=OP.add))
                    V(dve, lambda: nc.vector.reciprocal(out=den[:], in_=den[:]))
                    V(dve, lambda: nc.vector.tensor_tensor(out=t1[:], in0=abr[:], in1=lr, op=OP.mult))
                    V(dve, lambda: nc.vector.tensor_tensor(out=t2[:], in0=abi[:], in1=li, op=OP.mult))
                    V(dve, lambda: nc.vector.tensor_tensor(out=t1[:], in0=t1[:], in1=t2[:], op=OP.add))
                    V(dve, lambda: nc.vector.tensor_tensor(out=t1[:], in0=t1[:], in1=den[:], op=OP.mult))
                    V(dve, lambda: nc.vector.tensor_tensor(out=t2[:], in0=abi[:], in1=lr, op=OP.mult))
                    V(dve, lambda: nc.vector.tensor_tensor(out=abr[:], in0=abr[:], in1=li, op=OP.mult))
                    V(dve, lambda: nc.vector.tensor_tensor(out=t2[:], in0=t2[:], in1=abr[:], op=OP.subtract))
                    V(dve, lambda: nc.vector.tensor_tensor(out=t2[:], in0=t2[:], in1=den[:], op=OP.mult))
                    cr, ci = t1, t2
                    bxv = lambda ri: Bx[:, d_, :, :, ri, :].rearrange("p k g q -> p (k g) q")
                    t3, t4 = w[2], w[3]
                    V(dve, lambda: nc.vector.tensor_tensor(out=t3[:], in0=cr[:], in1=bb[:, 0, :], op=OP.mult))
                    V(dve, lambda: nc.vector.tensor_tensor(out=t4[:], in0=ci[:], in1=bb[:, 1, :], op=OP.mult))
                    P.op(dve, lambda: nc.vector.tensor_tensor(out=bxv(0), in0=v3(t3), in1=v3(t4), op=OP.subtract),
                         r=[R_w], w=[R_s5c])
                    V(dve, lambda: nc.vector.tensor_tensor(out=t3[:], in0=cr[:], in1=bb[:, 1, :], op=OP.mult))
                    V(dve, lambda: nc.vector.tensor_tensor(out=t4[:], in0=ci[:], in1=bb[:, 0, :], op=OP.mult))
                    P.op(dve, lambda: nc.vector.tensor_tensor(out=bxv(1), in0=v3(t3), in1=v3(t4), op=OP.add),
                         r=[R_w], w=[R_s5c])
                    angn = w[2]
                    V(dve, lambda: nc.vector.tensor_scalar(out=angn[:], in0=ang[:], scalar1=nvec[:, d_:d_ + 1],
                                                           scalar2=None, op0=OP.mult))
                    sin_of(sn, lambda: angn[:], 0.0, w[5])
                    sin_of(cs, lambda: angn[:], math.pi / 2, w[5])
                    pm, tm = w[5], w[6]
                    V(act, lambda: nc.scalar.activation(out=pm[:], in_=lnmag[:], func=AF.Exp, scale=nvec[:, d_:d_ + 1]))
                    V(act, lambda: nc.scalar.activation(out=tm[:], in_=lnmag[:], func=AF.Exp, scale=nneg[:, d_:d_ + 1]))
                    Wt = lambda fn: P.op(dve, fn, r=[R_w], w=[R_s5c])
                    Wt(lambda: nc.vector.tensor_tensor(out=tabs[:, 0, dc], in0=tm[:], in1=cs[:], op=OP.mult))
                    Wt(lambda: nc.vector.scalar_tensor_tensor(out=tabs[:, 1, dc], in0=tm[:], scalar=-1.0, in1=sn[:],
                                                              op0=OP.mult, op1=OP.mult))
                    Wt(lambda: nc.vector.tensor_tensor(out=tabs[:, 2, dc], in0=pm[:], in1=cs[:], op=OP.mult))
                    Wt(lambda: nc.vector.tensor_tensor(out=tabs[:, 3, dc], in0=pm[:], in1=sn[:], op=OP.mult))
                P.barrier()
            return tabs, Bx, Cst, wglu, R_s5c

        P.barrier()
        for si, (t0, L) in enumerate(seqs):
            ntile = L // 128
            nblk = L // 512
            b0 = t0 // 512
            with ExitStack() as es:
                xt_rot = Rot([scoped(es, "xt", [128, D]) for i in range(2)])
                xn_rot = Rot([scoped(es, "xn", [128, D]) for i in range(2)])
                junk = scoped(es, "junk", [128, D])
                R_junk = Res()
                for ti in range(ntile):
                    xt, xr = xt_rot.next()
                    P.dma(sp, xt[:], x_src[t0 + ti * 128:t0 + (ti + 1) * 128, :], w=[xr])
                    st, sr = rms_rstd(xt[:], xr, junk[:], R_junk)
                    xn, xnr = xn_rot.next()
                    P.op(pool, lambda: nc.gpsimd.tensor_scalar(out=xn[:], in0=xt[:], scalar1=st[:, 3:4], scalar2=None,
                                                              op0=OP.mult), r=[xr, sr], w=[xnr])
                    for hh in range(2):
                        pt, pr = psF.next()

                        def tr():
                            ins = None
                            for q in range(4):
                                fc = hh * 4 + q
                                ins = nc.tensor.transpose(out=pt[:, q * 128:(q + 1) * 128],
                                                          in_=xn[:, fc * 128:(fc + 1) * 128], identity=ident[:])
                            return ins
                        P.op(pe, tr, r=[xnr, R_const], w=[pr])
                        for q in range(4):
                            fc = hh * 4 + q
                            if q % 2 == 0:
                                P.op(act, lambda: nc.scalar.activation(
                                    out=hT[:, fc, ti * 128:(ti + 1) * 128], in_=pt[:, q * 128:(q + 1) * 128],
                                    func=AF.Identity, scale=sc1s[:, l, fc, si:si + 1],
                                    bias=mod_fm[:, l, 0, fc, si:si + 1]), r=[pr], w=[R_hT[ti]])
                            else:
                                P.op(dve, lambda: nc.vector.tensor_scalar(
                                    out=hT[:, fc, ti * 128:(ti + 1) * 128], in0=pt[:, q * 128:(q + 1) * 128],
                                    scalar1=sc1s[:, l, fc, si:si + 1], scalar2=mod_fm[:, l, 0, fc, si:si + 1],
                                    op0=OP.mult, op1=OP.add), r=[pr], w=[R_hT[ti]])
                P.barrier()

            def store_mix(tile_ap, tres, c, b, tok0, ntok):
                P.dma(sp, mixT_d[b0 + b, :, c, tok0:tok0 + ntok], tile_ap, r=[tres], w=[R_mixd[c][b0 + b]])

            with ExitStack() as es:
                mo_rot = Rot([scoped(es, "mo", [128, 512], BF16) for i in range(3)])
                if en_conv:
                    tbuf = scoped(es, "tbuf", [128, LMAX + 2])
                    R_t = Res()
                    cva = scoped(es, "cva", [128, 512])
                    R_cva = Res()
                    cvb_rot = Rot([scoped(es, "cvb", [128, 512]) for i in range(2)])
                    for i in range(2):
                        wx, wxr = load_win(2 + i)
                        wc, wcr = load_win(6 + i)
                        P.op(pool, lambda: nc.gpsimd.memset(tbuf[:, 0:1], 0.0), w=[R_t])
                        P.op(pool, lambda: nc.gpsimd.memset(tbuf[:, L + 1:L + 2], 0.0), w=[R_t])
                        for b in range(nblk):
                            p1, p1r = proj_block(wx, wxr, b)
                            p2, p2r = proj_block(wc, wcr, b)
                            P.op(act, lambda: nc.scalar.copy(out=cva[:], in_=p1[:, :]), r=[p1r], w=[R_cva])
                            P.op(dve, lambda: nc.vector.tensor_tensor(out=tbuf[:, 1 + b * 512:1 + (b + 1) * 512],
                                                                      in0=cva[:], in1=p2[:, :], op=OP.mult),
                                 r=[R_cva, p2r], w=[R_t])
                        wb, wbr = load_win(4 + i)
                        for b in range(nblk):
                            p3, p3r = proj_block(wb, wbr, b)
                            cb_, cbr = cvb_rot.next()
                            s0 = b * 512
                            P.op(pool, lambda: nc.gpsimd.tensor_scalar(out=cb_[:], in0=tbuf[:, s0:s0 + 512],
                                                                      scalar1=cw[:, l, i, 0:1], scalar2=None,
                                                                      op0=OP.mult), r=[R_t], w=[cbr])
                            P.op(dve, lambda: nc.vector.scalar_tensor_tensor(
                                out=cb_[:], in0=tbuf[:, s0 + 1:s0 + 513], scalar=cw[:, l, i, 1:2], in1=cb_[:],
                                op0=OP.mult, op1=OP.add), r=[R_t, cbr], w=[cbr])
                            P.op(dve, lambda: nc.vector.scalar_tensor_tensor(
                                out=cb_[:], in0=tbuf[:, s0 + 2:s0 + 514], scalar=cw[:, l, i, 2:3], in1=cb_[:],
                                op0=OP.mult, op1=OP.add), r=[R_t, cbr], w=[cbr])
                            mo, mor = mo_rot.next()
                            P.op(dve, lambda: nc.vector.tensor_tensor(out=mo[:], in0=cb_[:], in1=p3[:, :], op=OP.mult),
                                 r=[cbr, p3r], w=[mor])
                            store_mix(mo[:], mor, 2 + i, b, 0, 512)
                zero_chunks = ([] if en_conv else [2, 3]) + ([] if en_s5 else [0, 1]) + ([] if en_gdn else [4, 5, 6, 7])
                if zero_chunks:
                    mo, mor = mo_rot.next()
                    P.op(pool, lambda: nc.gpsimd.memset(mo[:], 0.0), w=[mor])
                    for c in zero_chunks:
                        for b in range(nblk):
                            store_mix(mo[:], mor, c, b, 0, 512)
                P.barrier()

            if en_s5:
                with ExitStack() as es:
                    tabs, Bx, Cst, wglu, R_s5c = s5_setup(es)
                    ub_rot = Rot([scoped(es, "ub", [128, 2, 512], BF16) for i in range(2)])
                    h_rot = Rot([scoped(es, "hs5", [128, 2048]) for i in range(3)])
                    z_rot = Rot([scoped(es, "zq", [128, 512], BF16) for i in range(2)])
                    ma_rot = Rot([scoped(es, "ma", [128, 512]) for i in range(2)])
                    mb_rot = Rot([scoped(es, "mb", [128, 512]) for i in range(2)])
                    hst_rot = Rot([scoped(es, "hst", [128, 16, 128], BF16) for i in range(2)])
                    yf_rot = Rot([scoped(es, "yf", [128, 256]) for i in range(2)])
                    yg_rot = Rot([scoped(es, "yg", [128, 2, 128]) for i in range(2)])
                    ygb_rot = Rot([scoped(es, "ygb", [128, 2, 128], BF16) for i in range(2)])
                    yt_rot = Rot([scoped(es, "yt", [128, 2, 128]) for i in range(2)])
                    mo2_rot = Rot([scoped(es, "mo2", [128, 2, 128], BF16) for i in range(2)])
                    wu0, wu0r = load_win(0)
                    wu1, wu1r = load_win(1)
                    for d_ in range(2):
                        order = list(range(ntile)) if d_ == 0 else list(range(ntile - 1, -1, -1))
                        tri = triF if d_ == 0 else triB
                        selm = selF if d_ == 0 else selB
                        hprev, hprev_r = None, None
                        ub, ubr, ub_blk = None, None, -1
                        for idx, ti in enumerate(order):
                            b = ti // 4
                            if b != ub_blk:
                                ub, ubr = ub_rot.next()
                                ub_blk = b
                                for c_, (wu, wur) in enumerate(((wu0, wu0r), (wu1, wu1r))):
                                    pu, pur = proj_block(wu, wur, b)
                                    P.op(act, lambda: nc.scalar.copy(out=ub[:, c_, :], in_=pu[:, :]), r=[pur], w=[ubr])
                            tk = slice((ti % 4) * 128, (ti % 4) * 128 + 128)
                            hcur, hcur_r = h_rot.next()
                            hst, hst_r = hst_rot.next()
                            for q in range(4):
                                kc2 = q // 2
                                gsl = slice(4 * (q % 2), 4 * (q % 2) + 4)
                                g16 = slice(4 * q, 4 * q + 4)
                                X, Xr = psF.next()
                                P.op(pe, lambda: nc.tensor.matmul(
                                    X[:, :], lhsT=ub[:, kc2, tk],
                                    rhs=Bx[:, d_, kc2, gsl, :, :].rearrange("p g r q -> p (g r q)"),
                                    start=True, stop=True), r=[ubr, R_s5c], w=[Xr])

                                def cmul(src, src_r, tr_i, ti_i, out_ap, out_r):
                                    s4 = src[:, :].rearrange("p (g r q) -> p g r q", g=4, r=2)
                                    Tr = tabs[:, tr_i, :].rearrange("p (a g q) -> p a g q", a=2, g=16)[:, d_, g16, :]
                                    Ti = tabs[:, ti_i, :].rearrange("p (a g q) -> p a g q", a=2, g=16)[:, d_, g16, :]
                                    ma, mar = ma_rot.next()
                                    mb, mbr = mb_rot.next()
                                    ma4 = ma[:].rearrange("p (g r q) -> p g r q", g=4, r=2)
                                    mb4 = mb[:].rearrange("p (g r q) -> p g r q", g=4, r=2)
                                    P.op(dve, lambda: nc.vector.tensor_tensor(
                                        out=ma4, in0=s4, in1=Tr.unsqueeze(2).to_broadcast([128, 4, 2, 64]), op=OP.mult),
                                        r=[src_r, R_s5c], w=[mar])
                                    P.op(dve, lambda: nc.vector.tensor_tensor(out=mb4[:, :, 0, :], in0=s4[:, :, 1, :],
                                                                              in1=Ti, op=OP.mult),
                                         r=[src_r, R_s5c], w=[mbr])
                                    P.op(dve, lambda: nc.vector.tensor_tensor(out=mb4[:, :, 1, :], in0=s4[:, :, 0, :],
                                                                              in1=Ti, op=OP.mult),
                                         r=[src_r, R_s5c], w=[mbr])
                                    o4 = out_ap.rearrange("p (g r q) -> p g r q", g=4, r=2)
                                    P.op(pool, lambda: nc.gpsimd.tensor_tensor(out=o4[:, :, 0, :], in0=ma4[:, :, 0, :],
                                                                              in1=mb4[:, :, 0, :], op=OP.subtract),
                                         r=[mar, mbr], w=[out_r])
                                    P.op(pool, lambda: nc.gpsimd.tensor_tensor(out=o4[:, :, 1, :], in0=ma4[:, :, 1, :],
                                                                              in1=mb4[:, :, 1, :], op=OP.add),
                                         r=[mar, mbr], w=[out_r])
                                zq, zqr = z_rot.next()
                                cmul(X, Xr, 0, 1, zq[:], zqr)
                                CZ, CZr = psF.next()

                                def mmc():
                                    ins = nc.tensor.matmul(CZ[:, :], lhsT=tri[:], rhs=zq[:], start=True, stop=(idx == 0))
                                    if idx > 0:
                                        ins = nc.tensor.matmul(CZ[:, :], lhsT=selm[:], rhs=hprev[:, q * 512:(q + 1) * 512],
                                                               start=False, stop=True)
                                    return ins
                                P.op(pe, mmc, r=[zqr, R_const] + ([hprev_r] if idx > 0 else []), w=[CZr])
                                cmul(CZ, CZr, 2, 3, hcur[:, q * 512:(q + 1) * 512], hcur_r)
                                pt, pr = psF.next()

                                def trh():
                                    ins = None
                                    for j in range(4):
                                        ins = nc.tensor.transpose(out=pt[:, j * 128:(j + 1) * 128],
                                                                  in_=hcur[:, q * 512 + j * 128:q * 512 + (j + 1) * 128],
                                                                  identity=ident[:])
                                    return ins
                                P.op(pe, trh, r=[hcur_r, R_const], w=[pr])
                                P.op(act, lambda: nc.scalar.copy(
                                    out=hst[:, 4 * q:4 * q + 4, :].rearrange("p g t -> p (g t)"), in_=pt[:, :]),
                                    r=[pr], w=[hst_r])
                            yp, ypr = psF.next()

                            def mmy():
                                ins = None
                                for kc2 in range(2):
                                    for gl in range(8):
                                        ins = nc.tensor.matmul(yp[:, kc2 * 128:(kc2 + 1) * 128],
                                                               lhsT=Cst[:, d_, kc2 * 8 + gl, :], rhs=hst[:, kc2 * 8 + gl, :],
                                                               start=(gl == 0), stop=(gl == 7))
                                return ins
                            P.op(pe, mmy, r=[hst_r, R_s5c], w=[ypr])
                            yf, yfr = yf_rot.next()
                            if d_ == 0:
                                P.op(act, lambda: nc.scalar.copy(out=yf[:], in_=yp[:, 0:256]), r=[ypr], w=[yfr])
                                P.dma(sp, yacc_d[b0 * 4 + ti], yf[:], r=[yfr], w=[R_yacc[b0 * 4 + ti]])
                            else:
                                P.dma(sp, yf[:], yacc_d[b0 * 4 + ti], r=[R_yacc[b0 * 4 + ti]], w=[yfr])
                                yg, ygr = yg_rot.next()
                                yt, ytr = yt_rot.next()
                                ygb, ygbr = ygb_rot.next()
                                yg2 = yg[:].rearrange("p a t -> p (a t)")
                                yt2 = yt[:].rearrange("p a t -> p (a t)")
                                P.op(dve, lambda: nc.vector.tensor_tensor(out=yg2, in0=yp[:, 0:256], in1=yf[:], op=OP.add),
                                     r=[ypr, yfr], w=[ygr])
                                for kc2 in range(2):
                                    P.op(dve, lambda: nc.vector.scalar_tensor_tensor(
                                        out=yg[:, kc2, :], in0=ub[:, kc2, tk], scalar=s5v[:, l, 0, kc2:kc2 + 1],
                                        in1=yg[:, kc2, :], op0=OP.mult, op1=OP.add), r=[ubr, ygr], w=[ygr])
                                c1 = 2.0 * math.sqrt(2.0 / math.pi)
                                P.op(pool, lambda: nc.gpsimd.tensor_tensor(out=yt2, in0=yg2, in1=yg2, op=OP.mult),
                                     r=[ygr], w=[ytr])
                                P.op(dve, lambda: nc.vector.tensor_scalar(out=yt2, in0=yt2, scalar1=c1 * 0.044715, scalar2=c1,
                                                                          op0=OP.mult, op1=OP.add), r=[ytr], w=[ytr])
                                P.op(pool, lambda: nc.gpsimd.tensor_tensor(out=yt2, in0=yt2, in1=yg2, op=OP.mult),
                                     r=[ygr, ytr], w=[ytr])
                                P.op(act, lambda: nc.scalar.activation(out=yt2, in_=yt2, func=AF.Sigmoid), r=[ytr], w=[ytr])
                                P.op(dve, lambda: nc.vector.tensor_tensor(out=yg2, in0=yg2, in1=yt2, op=OP.mult),
                                     r=[ygr, ytr], w=[ygr])
                                P.op(act, lambda: nc.scalar.copy(out=ygb[:], in_=yg[:]), r=[ygr], w=[ygbr])
                                pg, pgr = psF.next()

                                def mmg():
                                    ins = None
                                    for co in range(2):
                                        for kc2 in range(2):
                                            ins = nc.tensor.matmul(pg[:, co * 128:(co + 1) * 128],
                                                                   lhsT=wglu[:, kc2, co * 128:(co + 1) * 128],
                                                                   rhs=ygb[:, kc2, :], start=(kc2 == 0), stop=(kc2 == 1))
                                    return ins
                                P.op(pe, mmg, r=[ygbr, R_s5c], w=[pgr])
                                for co in range(2):
                                    P.op(act, lambda: nc.scalar.activation(
                                        out=yt[:, co, :], in_=pg[:, co * 128:(co + 1) * 128], func=AF.Sigmoid,
                                        bias=s5v[:, l, 1, co:co + 1], scale=1.0), r=[pgr], w=[ytr])
                                mo2, mo2r = mo2_rot.next()
                                P.op(dve, lambda: nc.vector.tensor_tensor(out=mo2[:], in0=yg[:], in1=yt[:], op=OP.mult),
                                     r=[ygr, ytr], w=[mo2r])
                                for co in range(2):
                                    store_mix(mo2[:, co, :], mo2r, co, b, (ti % 4) * 128, 128)
                            hprev, hprev_r = hcur, hcur_r
                    P.barrier()


            if en_gdn:
                with ExitStack() as es:
                    S2 = lambda name, shape, dt=F32: scoped(es, name, shape, dt)
                    raw = S2("graw", [128, LMAX + 2])
                    R_raw = Res()
                    qT = S2("gqT", [128, LMAX], BF16)
                    kT = S2("gkT", [128, LMAX], BF16)
                    ktok = S2("gktok", [128, LMAX // 128, 128], BF16)
                    vtok = S2("gvtok", [128, LMAX // 128, 128], BF16)
                    R_q = [Res() for _ in range(ntile)]
                    R_k = [Res() for _ in range(ntile)]
                    R_kt = [Res() for _ in range(ntile)]
                    R_vt = [Res() for _ in range(ntile)]
                    NTL = LMAX // 128
                    graw = S2("ggraw", [128, NTL, 16])
                    G_all = S2("gG", [128, NTL, 8])
                    B_all = S2("gB", [128, NTL, 8])
                    gc_all = S2("ggc", [128, NTL, 8])
                    gs_all = S2("ggs", [128, NTL, 8])
                    egc = S2("gegc", [128, NTL, 8])
                    egl = S2("gegl", [128, NTL, 8])
                    eke = S2("geke", [128, NTL, 8])
                    bge = S2("gbge", [128, NTL, 8])
                    gpar = S2("gpar", [128, 16])
                    gnw = S2("ggnw", [128, 128])
                    wg = S2("gwg", [128, KC, 16], BF16)
                    wz = S2("gwz", [128, KC, 512], BF16)
                    R_g = Res()
                    c_rot = Rot([S2("gc%d" % i, [128, 512]) for i in range(2)])
                    c2_rot = Rot([S2("gcc%d" % i, [128, 512]) for i in range(2)])
                    cb_rot = Rot([S2("gcb%d" % i, [128, 512], BF16) for i in range(2)])
                    m_rot = Rot([S2("gm%d" % i, [128, 128]) for i in range(24)])
                    mb_rot = Rot([S2("gmb%d" % i, [128, 128], BF16) for i in range(14)])
                    S_f = S2("gS", [128, 128])
                    S_b = S2("gSb", [128, 128], BF16)
                    R_S = Res()
                    o_rot = Rot([S2("go%d" % i, [128, 512]) for i in range(2)])
                    zs_rot = Rot([S2("gzs%d" % i, [128, 512]) for i in range(2)])
                    yb_rot = Rot([S2("gyb%d" % i, [128, 512], BF16) for i in range(2)])
                    mo4_rot = Rot([S2("gmo%d" % i, [128, 4, 128], BF16) for i in range(2)])
                    gjunk = S2("gjunk", [128, 128])
                    R_gj = Res()
                    P.dma(sp, gpar[:], gdn_gate[l, :].partition_broadcast(128), w=[R_g])
                    P.dma(sp, gnw[:], gdn_nw[l, :].partition_broadcast(128), w=[R_g])
                    P.dma(pool, wg[:], w_g[l].rearrange("(kc p) n -> p kc n", p=128), w=[R_g])
                    P.dma(pool, wz[:], w_z[l].rearrange("(kc p) n -> p kc n", p=128), w=[R_g])
                    Gop = lambda e, fn: P.op(e, fn, r=[R_g, R_const], w=[R_g])
                    Gop(act, lambda: nc.scalar.activation(out=gpar[:, 0:8], in_=gpar[:, 0:8], func=AF.Exp))
                    Gop(dve, lambda: nc.vector.tensor_scalar(out=gpar[:, 0:8], in0=gpar[:, 0:8], scalar1=-1.0, scalar2=None,
                                                             op0=OP.mult))
                    for ti in range(ntile):
                        pg, pgr = psF.next()

                        def mmg():
                            ins = None
                            for kc in range(KC):
                                ins = nc.tensor.matmul(pg[:, 0:16], lhsT=hT[:, kc, ti * 128:(ti + 1) * 128], rhs=wg[:, kc, :],
                                                       start=(kc == 0), stop=(kc == KC - 1))
                            return ins
                        P.op(pe, mmg, r=[R_g, R_hT[ti]], w=[pgr])
                        P.op(act, lambda: nc.scalar.copy(out=graw[:, ti, :], in_=pg[:, 0:16]), r=[pgr], w=[R_g])
                    nt8 = [128, ntile, 8]
                    Gv = lambda t: t[:, 0:ntile, :]
                    Gop(dve, lambda: nc.vector.tensor_tensor(out=Gv(G_all), in0=graw[:, 0:ntile, 0:8],
                                                             in1=gpar[:, 8:16].unsqueeze(1).to_broadcast(nt8), op=OP.add))
                    Gop(act, lambda: nc.scalar.activation(out=Gv(G_all), in_=Gv(G_all), func=AF.Exp))
                    Gop(dve, lambda: nc.vector.tensor_scalar(out=Gv(G_all), in0=Gv(G_all), scalar1=1.0, scalar2=None,
                                                             op0=OP.add))
                    Gop(act, lambda: nc.scalar.activation(out=Gv(G_all), in_=Gv(G_all), func=AF.Ln))
                    Gop(dve, lambda: nc.vector.tensor_tensor(out=Gv(G_all), in0=Gv(G_all),
                                                             in1=gpar[:, 0:8].unsqueeze(1).to_broadcast(nt8), op=OP.mult))
                    Gop(act, lambda: nc.scalar.activation(out=Gv(B_all), in_=graw[:, 0:ntile, 8:16], func=AF.Sigmoid))
                    for d_ in range(2):
                        pc, pcr = psF.next()
                        pcv = pc[:, 0:ntile * 4].rearrange("p (t c) -> p t c", c=4)
                        P.op(pe, lambda: nc.tensor.matmul(pcv, lhsT=(triF32 if d_ == 0 else triB32)[:],
                                                          rhs=G_all[:, 0:ntile, d_ * 4:d_ * 4 + 4], start=True, stop=True),
                             r=[R_g, R_const], w=[pcr])
                        P.op(act, lambda: nc.scalar.copy(out=gc_all[:, 0:ntile, d_ * 4:d_ * 4 + 4], in_=pcv), r=[pcr], w=[R_g])
                    pc, pcr = psF.next()
                    pcv = pc[:, 0:ntile * 8].rearrange("p (t c) -> p t c", c=8)
                    P.op(pe, lambda: nc.tensor.matmul(pcv, lhsT=ones_f[:], rhs=Gv(G_all), start=True, stop=True),
                         r=[R_g, R_const], w=[pcr])
                    P.op(act, lambda: nc.scalar.copy(out=Gv(gs_all), in_=pcv), r=[pcr], w=[R_g])
                    Gop(act, lambda: nc.scalar.activation(out=Gv(egc), in_=Gv(gc_all), func=AF.Exp))
                    Gop(act, lambda: nc.scalar.activation(out=Gv(egl), in_=Gv(gs_all), func=AF.Exp))
                    Gop(dve, lambda: nc.vector.tensor_tensor(out=Gv(eke), in0=Gv(gs_all), in1=Gv(gc_all), op=OP.subtract))
                    Gop(act, lambda: nc.scalar.activation(out=Gv(eke), in_=Gv(eke), func=AF.Exp))
                    Gop(dve, lambda: nc.vector.tensor_tensor(out=Gv(bge), in0=Gv(B_all), in1=Gv(egc), op=OP.mult))

                    GST = int(os.environ.get('GDN_STAGE', '3'))
                    for h in range(4 if GST >= 1 else 0):
                        for comp in range(3):
                            chunk = 8 + 4 * comp + h
                            wt, wtr = load_win(chunk)
                            P.op(pool, lambda: nc.gpsimd.memset(raw[:, 0:1], 0.0), w=[R_raw])
                            P.op(pool, lambda: nc.gpsimd.memset(raw[:, L + 1:L + 2], 0.0), w=[R_raw])
                            for b in range(nblk):
                                pp, ppr = proj_block(wt, wtr, b)
                                P.op(act, lambda: nc.scalar.copy(out=raw[:, 1 + b * 512:1 + (b + 1) * 512], in_=pp[:, :]),
                                     r=[ppr], w=[R_raw])
                            ci_ = 4 * comp + h
                            for b in range(nblk):
                                s0 = b * 512
                                cv, cvr = c_rot.next()
                                P.op(pool, lambda: nc.gpsimd.tensor_scalar(out=cv[:], in0=raw[:, s0:s0 + 512],
                                                                          scalar1=gcw[:, l, ci_, 0:1], scalar2=None,
                                                                          op0=OP.mult), r=[R_raw, R_const], w=[cvr])
                                P.op(dve, lambda: nc.vector.scalar_tensor_tensor(
                                    out=cv[:], in0=raw[:, s0 + 1:s0 + 513], scalar=gcw[:, l, ci_, 1:2], in1=cv[:],
                                    op0=OP.mult, op1=OP.add), r=[R_raw, cvr], w=[cvr])
                                P.op(dve, lambda: nc.vector.scalar_tensor_tensor(
                                    out=cv[:], in0=raw[:, s0 + 2:s0 + 514], scalar=gcw[:, l, ci_, 2:3], in1=cv[:],
                                    op0=OP.mult, op1=OP.add), r=[R_raw, cvr], w=[cvr])
                                tiles4 = list(range(b * 4, b * 4 + 4))
                                if comp == 2:
                                    cbf, cbfr = cb_rot.next()
                                    P.op(act, lambda: nc.scalar.activation(out=cbf[:], in_=cv[:], func=AF.Silu), r=[cvr], w=[cbfr])
                                    srcb, srcr = cbf, cbfr
                                else:
                                    P.op(act, lambda: nc.scalar.activation(out=cv[:], in_=cv[:], func=AF.Silu), r=[cvr], w=[cvr])
                                    c2, c2r = c2_rot.next()
                                    P.op(pool, lambda: nc.gpsimd.tensor_tensor(out=c2[:], in0=cv[:], in1=cv[:], op=OP.mult),
                                         r=[cvr], w=[c2r])
                                    pss, pssr = psF.next()
                                    P.op(pe, lambda: nc.tensor.matmul(pss[:, :], lhsT=ones_f[:], rhs=c2[:], start=True, stop=True),
                                         r=[c2r, R_const], w=[pssr])
                                    P.op(dve, lambda: nc.vector.tensor_scalar(out=c2[:], in0=pss[:, :], scalar1=EPS, scalar2=None,
                                                                              op0=OP.add), r=[pssr], w=[c2r])
                                    P.op(act, lambda: nc.scalar.activation(out=c2[:], in_=c2[:], func=AF.Ln), r=[c2r], w=[c2r])
                                    P.op(act, lambda: nc.scalar.activation(out=c2[:], in_=c2[:], func=AF.Exp, scale=-0.5),
                                         r=[c2r], w=[c2r])
                                    dstT = qT if comp == 0 else kT
                                    dres = R_q if comp == 0 else R_k
                                    sc_ = (128.0 ** -0.5) if comp == 0 else 1.0
                                    P.op(dve, lambda: nc.vector.scalar_tensor_tensor(
                                        out=dstT[:, s0:s0 + 512], in0=cv[:], scalar=sc_, in1=c2[:], op0=OP.mult, op1=OP.mult),
                                        r=[cvr, c2r], w=[dres[t] for t in tiles4])
                                    srcb, srcr = None, None
                                if comp >= 1:
                                    ph, phr = psH.next()

                                    def trk():
                                        ins = None
                                        for j in range(4):
                                            src_ap = (kT[:, s0 + j * 128:s0 + (j + 1) * 128] if comp == 1
                                                      else srcb[:, j * 128:(j + 1) * 128])
                                            ins = nc.tensor.transpose(out=ph[:, j * 128:(j + 1) * 128], in_=src_ap,
                                                                      identity=ident_bf[:])
                                        return ins
                                    rr = [R_k[t] for t in tiles4] if comp == 1 else [srcr]
                                    P.op(pe, trk, r=rr + [R_const], w=[phr])
                                    dtok = ktok if comp == 1 else vtok
                                    dtr = R_kt if comp == 1 else R_vt
                                    P.op(act, lambda: nc.scalar.copy(
                                        out=dtok[:, b * 4:b * 4 + 4, :].rearrange("p t d -> p (t d)"), in_=ph[:, 0:512]),
                                        r=[phr], w=[dtr[t] for t in tiles4])
                        for d_ in range(2 if GST >= 2 else 0):
                            c = d_ * 4 + h
                            order = list(range(ntile)) if d_ == 0 else list(range(ntile - 1, -1, -1))
                            P.op(dve, lambda: nc.vector.memset(S_f[:], 0.0), w=[R_S])
                            P.op(dve, lambda: nc.vector.memset(S_b[:], 0.0), w=[R_S])
                            for ti in order:
                                if not hasattr(P, 'rec0'):
                                    P.rec0 = P.ninst
                                tk = slice(ti * 128, (ti + 1) * 128)
                                gcol = gc_all[:, ti, c:c + 1]
                                M = m_rot.next
                                MB = mb_rot.next
                                kq, kqr = psF.next()

                                def mmkq():
                                    nc.tensor.matmul(kq[:, 0:128], lhsT=kT[:, tk], rhs=kT[:, tk], start=True, stop=True)
                                    return nc.tensor.matmul(kq[:, 128:256], lhsT=kT[:, tk], rhs=qT[:, tk], start=True, stop=True)
                                P.op(pe, mmkq, r=[R_k[ti], R_q[ti]], w=[kqr])
                                dg, dgr = M()
                                P.op(dve, lambda: nc.vector.tensor_scalar(out=dg[:], in0=ident[:], scalar1=gcol, scalar2=None,
                                                                          op0=OP.mult), r=[R_g, R_const], w=[dgr])
                                gb, gbr = psF.next()
                                P.op(pe, lambda: nc.tensor.matmul(gb[:, 0:128], lhsT=ones_f[:], rhs=dg[:], start=True, stop=True),
                                     r=[dgr, R_const], w=[gbr])
                                X, Xr = M()
                                P.op(dve, lambda: nc.vector.tensor_scalar(out=X[:], in0=gb[:, 0:128], scalar1=-1.0, scalar2=gcol,
                                                                          op0=OP.mult, op1=OP.add), r=[gbr, R_g], w=[Xr])
                                e1, e1r = M()
                                e2, e2r = M()
                                P.op(pool, lambda: nc.gpsimd.tensor_scalar(out=e1[:], in0=X[:], scalar1=0.0, scalar2=None,
                                                                          op0=OP.min), r=[Xr], w=[e1r])
                                P.op(pool, lambda: nc.gpsimd.tensor_scalar(out=e2[:], in0=X[:], scalar1=-1.0, scalar2=0.0,
                                                                          op0=OP.mult, op1=OP.min), r=[Xr], w=[e2r])
                                P.op(act, lambda: nc.scalar.activation(out=e1[:], in_=e1[:], func=AF.Exp), r=[e1r], w=[e1r])
                                P.op(act, lambda: nc.scalar.activation(out=e2[:], in_=e2[:], func=AF.Exp), r=[e2r], w=[e2r])
                                P.op(pool, lambda: nc.gpsimd.tensor_tensor(out=e1[:], in0=e1[:], in1=gML[:, d_, :], op=OP.mult),
                                     r=[e1r, R_const], w=[e1r])
                                P.op(pool, lambda: nc.gpsimd.tensor_tensor(out=e2[:], in0=e2[:], in1=gMA[:, d_, :], op=OP.mult),
                                     r=[e2r, R_const], w=[e2r])
                                Lm, Lr = M()
                                P.op(dve, lambda: nc.vector.scalar_tensor_tensor(
                                    out=Lm[:], in0=kq[:, 0:128], scalar=B_all[:, ti, c:c + 1], in1=e1[:], op0=OP.mult, op1=OP.mult),
                                    r=[kqr, e1r, R_g], w=[Lr])
                                ATb, ATr = MB()
                                P.op(dve, lambda: nc.vector.tensor_tensor(out=ATb[:], in0=kq[:, 128:256], in1=e2[:], op=OP.mult),
                                     r=[kqr, e2r], w=[ATr])
                                CUT = int(os.environ.get('GDN_CUT', '9'))
                                if CUT <= 1:
                                    continue
                                Ld, Ldr = M()
                                Lo, Lor = M()
                                P.op(dve, lambda: nc.vector.tensor_tensor(out=Ld[:], in0=Lm[:], in1=gBD[:], op=OP.mult),
                                     r=[Lr, R_const], w=[Ldr])
                                P.op(dve, lambda: nc.vector.tensor_tensor(out=Lo[:], in0=Lm[:], in1=Ld[:], op=OP.subtract),
                                     r=[Lr, Ldr], w=[Lor])
                                SUB = int(os.environ.get('GDN_SUB', '9'))
                                if SUB <= 0:
                                    continue
                                pr_, prr = psF.next()
                                P.op(pe, lambda: nc.tensor.matmul(pr_[:, 0:128], lhsT=Ld[:], rhs=ident[:], start=True, stop=True),
                                     r=[Ldr, R_const], w=[prr])
                                Rd, Rdr = M()
                                U, Ur = M()
                                if SUB <= 1:
                                    continue
                                P.op(dve, lambda: nc.vector.tensor_copy(out=Rd[:], in_=pr_[:, 0:128]), r=[prr], w=[Rdr])
                                if SUB <= 2:
                                    continue
                                P.op(dve, lambda: nc.vector.tensor_tensor(out=U[:], in0=ident[:], in1=Rd[:], op=OP.subtract),
                                     r=[Rdr, R_const], w=[Ur])
                                Lp, Lpr, Rp, Rpr = Ld, Ldr, Rd, Rdr
                                for j in range(1, int(os.environ.get("GDN_NJ", "6"))):
                                    p2, p2r = psF.next()

                                    def mmsq():
                                        ins = nc.tensor.matmul(p2[:, 0:128], lhsT=Rp[:], rhs=Lp[:], start=True, stop=True)
                                        if j < 5:
                                            ins = nc.tensor.matmul(p2[:, 128:256], lhsT=Lp[:], rhs=Rp[:], start=True, stop=True)
                                        return ins
                                    P.op(pe, mmsq, r=[Lpr, Rpr], w=[p2r])
                                    L2, L2r = M()
                                    P.op(dve, lambda: nc.vector.tensor_copy(out=L2[:], in_=p2[:, 0:128]), r=[p2r], w=[L2r])
                                    if j < 5:
                                        R2, R2r = M()
                                        P.op(dve, lambda: nc.vector.tensor_copy(out=R2[:], in_=p2[:, 128:256]), r=[p2r], w=[R2r])
                                    else:
                                        R2, R2r = None, None
                                    if os.environ.get('GDN_NOU'):
                                        Lp, Lpr, Rp, Rpr = L2, L2r, R2, R2r
                                        continue
                                    p3, p3r = psF.next()
                                    P.op(pe, lambda: nc.tensor.matmul(p3[:, 0:128], lhsT=L2[:], rhs=U[:], start=True, stop=True),
                                         r=[L2r, Ur], w=[p3r])
                                    U2, U2r = M()
                                    P.op(dve, lambda: nc.vector.tensor_tensor(out=U2[:], in0=U[:], in1=p3[:, 0:128], op=OP.add),
                                         r=[Ur, p3r], w=[U2r])
                                    U, Ur = U2, U2r
                                    Lp, Lpr, Rp, Rpr = L2, L2r, R2, R2r
                                if CUT <= 2:
                                    continue
                                p4, p4r = psF.next()

                                def mmoff():
                                    nc.tensor.matmul(p4[:, 0:128], lhsT=U[:], rhs=ident[:], start=True, stop=True)
                                    return nc.tensor.matmul(p4[:, 128:256], lhsT=Lo[:], rhs=U[:], start=True, stop=True)
                                P.op(pe, mmoff, r=[Ur, Lor, R_const], w=[p4r])
                                Td, Tdr = M()
                                X1, X1r = M()
                                P.op(dve, lambda: nc.vector.tensor_copy(out=Td[:], in_=p4[:, 0:128]), r=[p4r], w=[Tdr])
                                P.op(dve, lambda: nc.vector.tensor_copy(out=X1[:], in_=p4[:, 128:256]), r=[p4r], w=[X1r])
                                p5, p5r = psF.next()
                                P.op(pe, lambda: nc.tensor.matmul(p5[:, 0:128], lhsT=Td[:], rhs=X1[:], start=True, stop=True),
                                     r=[Tdr, X1r], w=[p5r])
                                Ub, Ubr = MB()
                                P.op(dve, lambda: nc.vector.scalar_tensor_tensor(out=Ub[:], in0=p5[:, 0:128], scalar=-1.0, in1=U[:],
                                                                                 op0=OP.mult, op1=OP.add),
                                     r=[Ur, p5r], w=[Ubr])
                                if CUT <= 3:
                                    continue
                                bv, bvr = MB()
                                kbg, kbgr = MB()
                                kend, kendr = MB()
                                P.op(dve, lambda: nc.vector.tensor_scalar(out=bv[:], in0=vtok[:, ti, :], scalar1=B_all[:, ti, c:c + 1],
                                                                          scalar2=None, op0=OP.mult), r=[R_vt[ti], R_g], w=[bvr])
                                P.op(dve, lambda: nc.vector.tensor_scalar(out=kbg[:], in0=ktok[:, ti, :], scalar1=bge[:, ti, c:c + 1],
                                                                          scalar2=None, op0=OP.mult), r=[R_kt[ti], R_g], w=[kbgr])
                                P.op(dve, lambda: nc.vector.tensor_scalar(out=kend[:], in0=ktok[:, ti, :], scalar1=eke[:, ti, c:c + 1],
                                                                          scalar2=None, op0=OP.mult), r=[R_kt[ti], R_g], w=[kendr])
                                uw, uwr = psF.next()

                                def mmuw():
                                    nc.tensor.matmul(uw[:, 0:128], lhsT=Ub[:], rhs=bv[:], start=True, stop=True)
                                    return nc.tensor.matmul(uw[:, 128:256], lhsT=kbg[:], rhs=Ub[:], start=True, stop=True)
                                P.op(pe, mmuw, r=[Ubr, bvr, kbgr], w=[uwr])
                                us, usr = M()
                                wTb, wTr = MB()
                                P.op(dve, lambda: nc.vector.tensor_copy(out=us[:], in_=uw[:, 0:128]), r=[uwr], w=[usr])
                                P.op(dve, lambda: nc.vector.tensor_copy(out=wTb[:], in_=uw[:, 128:256]), r=[uwr], w=[wTr])
                                sq, sqr = psF.next()

                                def mmsq2():
                                    nc.tensor.matmul(sq[:, 0:128], lhsT=wTb[:], rhs=S_b[:], start=True, stop=True)
                                    return nc.tensor.matmul(sq[:, 128:256], lhsT=qT[:, tk], rhs=S_b[:], start=True, stop=True)
                                P.op(pe, mmsq2, r=[wTr, R_S, R_q[ti]], w=[sqr])
                                vnb, vnr = MB()
                                P.op(dve, lambda: nc.vector.scalar_tensor_tensor(out=vnb[:], in0=sq[:, 0:128], scalar=-1.0, in1=us[:],
                                                                                 op0=OP.mult, op1=OP.add),
                                     r=[usr, sqr], w=[vnr])
                                ak, akr = psF.next()

                                def mmak():
                                    nc.tensor.matmul(ak[:, 0:128], lhsT=ATb[:], rhs=vnb[:], start=True, stop=True)
                                    return nc.tensor.matmul(ak[:, 128:256], lhsT=kend[:], rhs=vnb[:], start=True, stop=True)
                                P.op(pe, mmak, r=[ATr, vnr, kendr], w=[akr])
                                o1, o1r = M()
                                gti = b0 * 4 + ti
                                if d_ == 0:
                                    P.op(dve, lambda: nc.vector.tensor_copy(out=o1[:], in_=ak[:, 0:128]), r=[akr], w=[o1r])
                                else:
                                    o0, o0r = M()
                                    P.dma(sp, o0[:], ofw_d[gti, :, h * 128:(h + 1) * 128], r=[R_ofw[gti]], w=[o0r])
                                    P.op(dve, lambda: nc.vector.tensor_tensor(out=o1[:], in0=o0[:], in1=ak[:, 0:128], op=OP.add),
                                         r=[o0r, akr], w=[o1r])
                                P.op(dve, lambda: nc.vector.scalar_tensor_tensor(
                                    out=o1[:], in0=sq[:, 128:256], scalar=egc[:, ti, c:c + 1], in1=o1[:], op0=OP.mult, op1=OP.add),
                                    r=[sqr, o1r, R_g], w=[o1r])
                                P.dma(sp, ofw_d[gti, :, h * 128:(h + 1) * 128], o1[:], r=[o1r], w=[R_ofw[gti]])
                                P.op(dve, lambda: nc.vector.scalar_tensor_tensor(
                                    out=S_f[:], in0=S_f[:], scalar=egl[:, ti, c:c + 1], in1=ak[:, 128:256], op0=OP.mult, op1=OP.add),
                                    r=[akr, R_g], w=[R_S])
                                P.op(dve, lambda: nc.vector.tensor_copy(out=S_b[:], in_=S_f[:]), r=[R_S], w=[R_S])
                    for ti in range(ntile if GST >= 3 else 0):
                        gti = b0 * 4 + ti
                        ot, otr = o_rot.next()
                        P.dma(sp, ot[:], ofw_d[gti], r=[R_ofw[gti]], w=[otr])
                        pz, pzr = psF.next()

                        def mmz():
                            ins = None
                            for kc in range(KC):
                                ins = nc.tensor.matmul(pz[:, :], lhsT=hT[:, kc, ti * 128:(ti + 1) * 128], rhs=wz[:, kc, :],
                                                       start=(kc == 0), stop=(kc == KC - 1))
                            return ins
                        P.op(pe, mmz, r=[R_g, R_hT[ti]], w=[pzr])
                        zs, zsr = zs_rot.next()
                        P.op(act, lambda: nc.scalar.activation(out=zs[:], in_=pz[:, :], func=AF.Silu), r=[pzr], w=[zsr])
                        st, sr = st_rot.next()
                        for hh in range(4):
                            P.op(act, lambda: nc.scalar.activation(out=gjunk[:], in_=ot[:, hh * 128:(hh + 1) * 128], func=AF.Square,
                                                                   accum_out=st[:, hh:hh + 1]), r=[otr], w=[R_gj, sr])
                        P.op(dve, lambda: nc.vector.tensor_scalar(out=st[:], in0=st[:], scalar1=1.0 / 128, scalar2=EPS,
                                                                  op0=OP.mult, op1=OP.add), r=[sr], w=[sr])
                        P.op(act, lambda: nc.scalar.activation(out=st[:], in_=st[:], func=AF.Sqrt), r=[sr], w=[sr])
                        P.op(dve, lambda: nc.vector.reciprocal(out=st[:], in_=st[:]), r=[sr], w=[sr])
                        for hh in range(4):
                            P.op(dve, lambda: nc.vector.scalar_tensor_tensor(
                                out=ot[:, hh * 128:(hh + 1) * 128], in0=ot[:, hh * 128:(hh + 1) * 128], scalar=st[:, hh:hh + 1],
                                in1=gnw[:], op0=OP.mult, op1=OP.mult), r=[otr, sr, R_g], w=[otr])
                        yb, ybr = yb_rot.next()
                        P.op(pool, lambda: nc.gpsimd.tensor_tensor(out=yb[:], in0=ot[:], in1=zs[:], op=OP.mult),
                             r=[otr, zsr], w=[ybr])
                        ph, phr = psH.next()

                        def try_():
                            ins = None
                            for j in range(4):
                                ins = nc.tensor.transpose(out=ph[:, j * 128:(j + 1) * 128], in_=yb[:, j * 128:(j + 1) * 128],
                                                          identity=ident_bf[:])
                            return ins
                        P.op(pe, try_, r=[ybr, R_const], w=[phr])
                        mo4, mo4r = mo4_rot.next()
                        P.op(act, lambda: nc.scalar.copy(out=mo4[:].rearrange("p h t -> p (h t)"), in_=ph[:, 0:512]),
                             r=[phr], w=[mo4r])
                        for hh in range(4):
                            store_mix(mo4[:, hh, :], mo4r, 4 + hh, ti // 4, (ti % 4) * 128, 128)
                    P.barrier()

            with ExitStack() as es:
                wout_sb = scoped(es, "wout_sb", [128, KC, D], BF16)
                R_wout = Res()
                g1b = scoped(es, "g1b", [128, D])
                R_bc = Res()
                xt_rot = Rot([scoped(es, "xt", [128, D]) for i in range(2)])
                xn_rot = Rot([scoped(es, "xn", [128, D]) for i in range(2)])
                mx_rot = Rot([scoped(es, "mx", [128, KC, 512], BF16) for i in range(2)])
                junk = scoped(es, "junk", [128, D])
                R_junk = Res()
                P.dma(pool, wout_sb[:], w_out[l].rearrange("(kc p) n -> p kc n", p=128), w=[R_wout])
                P.dma(sp, g1b[:], mod_row_d[si, l, 2 * D:3 * D].partition_broadcast(128), r=[R_modrow], w=[R_bc])
                if en_moe:
                    sc2b = scoped(es, "sc2b", [128, D])
                    sh2b = scoped(es, "sh2b", [128, D])
                    nffb = scoped(es, "nffb", [128, D])
                    h2_rot = Rot([scoped(es, "h2", [128, D]) for i in range(2)])
                    h2f_rot = Rot([scoped(es, "h2f", [128, KC, 128]) for i in range(2)])
                    h2b_rot = Rot([scoped(es, "h2b", [128, KC, 512], BF16) for i in range(2)])
                    wr_sb = scoped(es, "wr_sb", [128, KC, 36])
                    rbb = scoped(es, "rbb", [128, 36])
                    rt_rot = Rot([scoped(es, "rt", [128, 160]) for i in range(2)])
                    P.dma(sp, sc2b[:], mod_row_d[si, l, 4 * D:5 * D].partition_broadcast(128), r=[R_modrow], w=[R_bc])
                    P.dma(sp, sh2b[:], mod_row_d[si, l, 3 * D:4 * D].partition_broadcast(128), r=[R_modrow], w=[R_bc])
                    P.dma(sp, nffb[:], nffn_row[l, :].partition_broadcast(128), w=[R_bc])
                    P.dma(sp, wr_sb[:], moe_wr[l].rearrange("(kc p) n -> p kc n", p=128), w=[R_bc])
                    P.dma(sp, rbb[:], moe_rb[l, :].partition_broadcast(128), w=[R_bc])
                    P.op(dve, lambda: nc.vector.scalar_tensor_tensor(out=sc2b[:], in0=sc2b[:], scalar=1.0, in1=nffb[:],
                                                                     op0=OP.add, op1=OP.mult), r=[R_bc], w=[R_bc])
                    h2b, h2br = None, None
                mx, mxr = None, None
                for ti in range(ntile):
                    b = ti // 4
                    if ti % 4 == 0:
                        mx, mxr = mx_rot.next()
                        P.dma(act, mx[:], mixT_d[b0 + b], r=[R_mixd[c][b0 + b] for c in range(KC)], w=[mxr])
                    xt, xr = xt_rot.next()
                    P.dma(sp, xt[:], x_src[t0 + ti * 128:t0 + (ti + 1) * 128, :], w=[xr])
                    xn, xnr = xn_rot.next()
                    for half in range(2):
                        pt, pr = psF.next()

                        def mm():
                            ins = None
                            for kc in range(KC):
                                ins = nc.tensor.matmul(pt[:, :], lhsT=mx[:, kc, (ti % 4) * 128:(ti % 4) * 128 + 128],
                                                       rhs=wout_sb[:, kc, half * 512:(half + 1) * 512],
                                                       start=(kc == 0), stop=(kc == KC - 1))
                            return ins
                        P.op(pe, mm, r=[R_wout, mxr], w=[pr])
                        hs = slice(half * 512, (half + 1) * 512)
                        P.op(dve, lambda: nc.vector.tensor_tensor(out=xn[:, hs], in0=pt[:, :], in1=g1b[:, hs], op=OP.mult),
                             r=[pr, R_bc], w=[xnr])
                        P.op(pool, lambda: nc.gpsimd.tensor_tensor(out=xn[:, hs], in0=xn[:, hs], in1=xt[:, hs], op=OP.add),
                             r=[xnr, xr], w=[xnr])
                    if not en_moe and l == depth - 1:
                        st, sr = rms_rstd(xn[:], xnr, junk[:], R_junk)
                        P.op(dve, lambda: nc.vector.scalar_tensor_tensor(out=xn[:], in0=xn[:], scalar=st[:, 3:4],
                                                                         in1=finb[:], op0=OP.mult, op1=OP.mult),
                             r=[xnr, sr, R_const], w=[xnr])
                        P.dma(sp, y_out[t0 + ti * 128:t0 + (ti + 1) * 128, :], xn[:], r=[xnr])
                    else:
                        P.dma(sp, x_work[t0 + ti * 128:t0 + (ti + 1) * 128, :], xn[:], r=[xnr])
                    if en_moe:
                        gti = b0 * 4 + ti
                        st, sr = rms_rstd(xn[:], xnr, junk[:], R_junk)
                        h2, h2r = h2_rot.next()
                        P.op(pool, lambda: nc.gpsimd.tensor_scalar(out=h2[:], in0=xn[:], scalar1=st[:, 3:4], scalar2=None,
                                                                  op0=OP.mult), r=[xnr, sr], w=[h2r])
                        P.op(dve, lambda: nc.vector.tensor_tensor(out=h2[:], in0=h2[:], in1=sc2b[:], op=OP.mult),
                             r=[h2r, R_bc], w=[h2r])
                        P.op(pool, lambda: nc.gpsimd.tensor_tensor(out=h2[:], in0=h2[:], in1=sh2b[:], op=OP.add),
                             r=[h2r, R_bc], w=[h2r])
                        h2f, h2fr = h2f_rot.next()
                        if ti % 4 == 0:
                            h2b, h2br = h2b_rot.next()
                        for hh in range(2):
                            pt, pr = psF.next()

                            def tr2():
                                ins = None
                                for q in range(4):
                                    fc = hh * 4 + q
                                    ins = nc.tensor.transpose(out=pt[:, q * 128:(q + 1) * 128],
                                                              in_=h2[:, fc * 128:(fc + 1) * 128], identity=ident[:])
                                return ins
                            P.op(pe, tr2, r=[h2r, R_const], w=[pr])
                            P.op(dve, lambda: nc.vector.tensor_copy(
                                out=h2f[:, hh * 4:hh * 4 + 4, :].rearrange("p k t -> p (k t)"), in_=pt[:, :]), r=[pr], w=[h2fr])
                        P.op(act, lambda: nc.scalar.copy(out=h2b[:, :, (ti % 4) * 128:(ti % 4) * 128 + 128], in_=h2f[:]),
                             r=[h2fr], w=[h2br])
                        if ti % 4 == 3:
                            P.dma(act, h2T_d[b0 + ti // 4], h2b[:], r=[h2br], w=[R_h2d[b0 + ti // 4]])
                        pl, plr = psF.next()

                        def mmr():
                            ins = None
                            for kc in range(KC):
                                ins = nc.tensor.matmul(pl[:, 0:36], lhsT=h2f[:, kc, :], rhs=wr_sb[:, kc, :],
                                                       start=(kc == 0), stop=(kc == KC - 1))
                            return ins
                        P.op(pe, mmr, r=[h2fr, R_bc], w=[plr])
                        rt, rtr = rt_rot.next()
                        RT = lambda fn, e=dve: P.op(e, fn, r=[rtr], w=[rtr])
                        lg = rt[:, 0:36]
                        gl, el = rt[:, 0:4], rt[:, 4:36]
                        P.op(dve, lambda: nc.vector.tensor_tensor(out=lg, in0=pl[:, 0:36], in1=rbb[:], op=OP.add),
                             r=[plr, R_bc], w=[rtr])
                        gmax, gsum, gw = rt[:, 36:37], rt[:, 37:38], rt[:, 38:39]
                        ohg, pen, gex = rt[:, 40:44], rt[:, 44:48], rt[:, 48:52]
                        elm = rt[:, 52:84]
                        top8 = rt[:, 84:92]
                        oh1, oh2 = rt[:, 92:124], rt[:, 124:156]
                        dd, w1, w2 = rt[:, 156:157], rt[:, 157:158], rt[:, 158:159]
                        RT(lambda: nc.vector.tensor_reduce(out=gmax, in_=gl, axis=AX.X, op=OP.max))
                        RT(lambda: nc.vector.tensor_scalar(out=ohg, in0=gl, scalar1=gmax, scalar2=None, op0=OP.is_ge))
                        RT(lambda: nc.vector.tensor_scalar(out=gex, in0=gl, scalar1=gmax, scalar2=None, op0=OP.subtract))
                        RT(lambda: nc.scalar.activation(out=gex, in_=gex, func=AF.Exp), act)
                        RT(lambda: nc.vector.tensor_reduce(out=gsum, in_=gex, axis=AX.X, op=OP.add))
                        RT(lambda: nc.vector.reciprocal(out=gw, in_=gsum))
                        RT(lambda: nc.vector.tensor_scalar(out=pen, in0=ohg, scalar1=1e30, scalar2=-1e30, op0=OP.mult,
                                                           op1=OP.add))
                        RT(lambda: nc.vector.tensor_tensor(out=elm.rearrange("p (g j) -> p g j", j=8),
                                                           in0=el.rearrange("p (g j) -> p g j", j=8),
                                                           in1=pen.unsqueeze(2).to_broadcast([128, 4, 8]), op=OP.add))
                        RT(lambda: nc.vector.max(out=top8, in_=elm))
                        RT(lambda: nc.vector.tensor_scalar(out=oh1, in0=elm, scalar1=top8[:, 0:1], scalar2=None,
                                                           op0=OP.is_equal))
                        RT(lambda: nc.vector.tensor_scalar(out=oh2, in0=elm, scalar1=top8[:, 1:2], scalar2=None,
                                                           op0=OP.is_equal))
                        RT(lambda: nc.vector.tensor_tensor(out=dd, in0=top8[:, 1:2], in1=top8[:, 0:1], op=OP.subtract))
                        RT(lambda: nc.scalar.activation(out=dd, in_=dd, func=AF.Exp), act)
                        RT(lambda: nc.vector.tensor_scalar(out=w1, in0=dd, scalar1=1.0, scalar2=None, op0=OP.add))
                        RT(lambda: nc.vector.reciprocal(out=w1, in_=w1))
                        RT(lambda: nc.vector.tensor_tensor(out=w1, in0=w1, in1=gw, op=OP.mult))
                        RT(lambda: nc.vector.tensor_tensor(out=w2, in0=w1, in1=dd, op=OP.mult))
                        P.op(dve, lambda: nc.vector.tensor_scalar(out=Wall[:, gti, :], in0=oh1, scalar1=w1, scalar2=None,
                                                                  op0=OP.mult), r=[rtr], w=[R_W[gti]])
                        P.op(dve, lambda: nc.vector.scalar_tensor_tensor(out=Wall[:, gti, :], in0=oh2, scalar=w2,
                                                                         in1=Wall[:, gti, :], op0=OP.mult, op1=OP.add),
                             r=[rtr, R_W[gti]], w=[R_W[gti]])
                P.barrier()
        if en_moe:
            SBT = min(1024, LMAX)
            nsb = LT // SBT
            with ExitStack() as es:
                yacc = scoped(es, "yacc", [128, SBT // 128, D])
                R_y = [Res() for _ in range(SBT // 128)]
                wgu_rot = Rot([scoped(es, "wgu", [128, KC, 2 * FF], BF16) for i in range(2)])
                wd_rot = Rot([scoped(es, "wd", [128, 4, D], BF16) for i in range(2)])
                at_rot = Rot([scoped(es, "actT", [128, 4, 512], BF16) for i in range(2)])
                sg_rot = Rot([scoped(es, "sg", [128, 512]) for i in range(2)])
                g2b = [scoped(es, "g2b", [128, D]) for i in range(2)]
                xt_rot = Rot([scoped(es, "xt", [128, D]) for i in range(2)])
                junk = scoped(es, "junk", [128, D])
                R_junk = Res()
                R_g2 = Res()
                for si in range(2):
                    P.dma(sp, g2b[si][:], mod_row_d[si, l, 5 * D:6 * D].partition_broadcast(128), r=[R_modrow], w=[R_g2])
                for sbk in range(nsb):
                    nb_ = SBT // 512
                    for bb_ in range(nb_):
                        gb_ = sbk * nb_ + bb_
                        P.dma(sp, hT[:, :, bb_ * 512:(bb_ + 1) * 512], h2T_d[gb_], r=[R_h2d[gb_]],
                              w=R_hT[bb_ * 4:(bb_ + 1) * 4])
                    P.op(pool, lambda: nc.gpsimd.memset(yacc[:], 0.0), w=R_y)
                    for e in range(NEXP):
                        wgu, wgur = wgu_rot.next()
                        wd, wdr = wd_rot.next()
                        P.dma(pool, wgu[:], moe_wgu[l, e].rearrange("(kc p) n -> p kc n", p=128), w=[wgur])
                        P.dma(pool, wd[:], moe_wd[l, e].rearrange("(fc p) n -> p fc n", p=128), w=[wdr])
                        for bb_ in range(nb_):
                            actT, atr = at_rot.next()
                            for fc in range(4):
                                pg_, pgr_ = psF.next()
                                pu_, pur_ = psF.next()

                                def mmgu():
                                    ins = None
                                    for kc in range(KC):
                                        ins = nc.tensor.matmul(pg_[:, :], lhsT=wgu[:, kc, fc * 128:(fc + 1) * 128],
                                                               rhs=hT[:, kc, bb_ * 512:(bb_ + 1) * 512],
                                                               start=(kc == 0), stop=(kc == KC - 1))
                                    for kc in range(KC):
                                        ins = nc.tensor.matmul(pu_[:, :], lhsT=wgu[:, kc, FF + fc * 128:FF + (fc + 1) * 128],
                                                               rhs=hT[:, kc, bb_ * 512:(bb_ + 1) * 512],
                                                               start=(kc == 0), stop=(kc == KC - 1))
                                    return ins
                                P.op(pe, mmgu, r=[wgur] + R_hT[bb_ * 4:(bb_ + 1) * 4], w=[pgr_, pur_])
                                sg, sgr = sg_rot.next()
                                P.op(act, lambda: nc.scalar.activation(out=sg[:], in_=pg_[:, :], func=AF.Silu), r=[pgr_], w=[sgr])
                                P.op(dve, lambda: nc.vector.tensor_tensor(out=actT[:, fc, :], in0=sg[:], in1=pu_[:, :], op=OP.mult),
                                     r=[sgr, pur_], w=[atr])
                            for tt in range(4):
                                lt_ = bb_ * 4 + tt
                                gti = sbk * (SBT // 128) + lt_
                                for half in range(2):
                                    po, por = psF.next()

                                    def mmd():
                                        ins = None
                                        for fc in range(4):
                                            ins = nc.tensor.matmul(po[:, :], lhsT=actT[:, fc, tt * 128:(tt + 1) * 128],
                                                                   rhs=wd[:, fc, half * 512:(half + 1) * 512],
                                                                   start=(fc == 0), stop=(fc == 3))
                                        return ins
                                    P.op(pe, mmd, r=[atr, wdr], w=[por])
                                    hs = slice(half * 512, (half + 1) * 512)
                                    P.op(dve, lambda: nc.vector.scalar_tensor_tensor(
                                        out=yacc[:, lt_, hs], in0=po[:, :], scalar=Wall[:, gti, e:e + 1], in1=yacc[:, lt_, hs],
                                        op0=OP.mult, op1=OP.add), r=[por, R_W[gti], R_y[lt_]], w=[R_y[lt_]])
                    for lt_ in range(SBT // 128):
                        gti = sbk * (SBT // 128) + lt_
                        si = 0 if gti * 128 < LP else 1
                        xt, xr = xt_rot.next()
                        P.dma(sp, xt[:], x_work[gti * 128:(gti + 1) * 128, :], w=[xr])
                        P.op(pool, lambda: nc.gpsimd.tensor_tensor(out=yacc[:, lt_, :], in0=yacc[:, lt_, :], in1=g2b[si][:],
                                                                  op=OP.mult), r=[R_y[lt_], R_g2], w=[R_y[lt_]])
                        P.op(dve, lambda: nc.vector.tensor_tensor(out=xt[:], in0=xt[:], in1=yacc[:, lt_, :], op=OP.add),
                             r=[xr, R_y[lt_]], w=[xr])
                        if l == depth - 1:
                            st, sr = rms_rstd(xt[:], xr, junk[:], R_junk)
                            P.op(dve, lambda: nc.vector.scalar_tensor_tensor(out=xt[:], in0=xt[:], scalar=st[:, 3:4],
                                                                             in1=finb[:], op0=OP.mult, op1=OP.mult),
                                 r=[xr, sr, R_const], w=[xr])
                            P.dma(sp, y_out[gti * 128:(gti + 1) * 128, :], xt[:], r=[xr])
                        else:
                            P.dma(sp, x_work[gti * 128:(gti + 1) * 128, :], xt[:], r=[xr])
                    P.barrier()
        x_src = x_work
    P.finish()
    return nc, P


def make_in_maps(inp, LP, LS, depth, ncores):
    f = lambda a: np.ascontiguousarray(np.asarray(a, dtype=np.float32))
    w_in = f(inp["w_in"])[:depth]
    w_in_pad = np.zeros((depth, D, NCH * 128), np.float32)
    w_in_pad[:, :, :IN_COLS] = w_in
    w_in_c = np.ascontiguousarray(
        w_in_pad.reshape(depth, KC, 128, NCH, 128).transpose(0, 3, 2, 1, 4)).reshape(depth, NCH, 128, KC * 128)
    b_ada = f(inp["b_ada"])[:depth]
    b_ada_fm = np.ascontiguousarray(b_ada.reshape(depth, 6, KC, 128).transpose(3, 0, 1, 2))
    nmix_fm = np.ascontiguousarray(f(inp["norm_mix"])[:depth].reshape(depth, KC, 128).transpose(2, 0, 1))
    convw_fm = np.ascontiguousarray(f(inp["conv_w"])[:depth].reshape(depth, 3, 2, 128).transpose(3, 0, 2, 1))
    shared = {
        "w_ada": f(inp["w_ada"])[:depth], "b_ada_row": b_ada, "b_ada_fm": b_ada_fm, "nmix_fm": nmix_fm,
        "nffn_row": f(inp["norm_ffn"])[:depth], "w_in_c": w_in_c, "w_out": f(inp["w_out"])[:depth],
        "convw_fm": convw_fm, "fin_row": f(inp["final_norm"]).reshape(1, D),
    }
    lam = np.stack([f(inp["s5_lam_re"])[:depth].reshape(depth, 2048), f(inp["s5_lam_im"])[:depth].reshape(depth, 2048)], axis=1)
    shared["s5_lam"] = np.ascontiguousarray(lam)
    shared["s5_ls"] = np.ascontiguousarray(f(inp["s5_log_step"])[:depth].reshape(depth, 32))
    bblk = np.zeros((depth, 2, 8, 16, 2, 2, 8, 64), np.float32)
    for ri, nm in enumerate(("s5_b_re", "s5_b_im")):
        b = f(inp[nm])[:depth].reshape(depth, 2, 2, 8, 64, 16)
        for gl in range(8):
            bblk[:, ri, gl, :, :, :, gl, :] = b[:, :, :, gl, :, :].transpose(0, 4, 1, 2, 3)
    shared["s5_bblk"] = np.ascontiguousarray(bblk.reshape(depth, 2, 128, 2048))
    cst = np.zeros((depth, 2, 64, 2, 16, 8, 16), np.float32)
    for ri, nm in enumerate(("s5_c_re", "s5_c_im")):
        c = f(inp[nm])[:depth]
        for g in range(16):
            cst[:, ri, :, :, g, g % 8, :] = c[:, :, g, :, :].transpose(0, 3, 1, 2)
    shared["s5_cst"] = np.ascontiguousarray(cst.reshape(depth, 128, 2 * 16 * 128))
    shared["s5_wglu"] = f(inp["s5_w_glu"])[:depth]
    vec = np.stack([f(inp["s5_d"])[:depth].reshape(depth, 2, 128), f(inp["s5_b_glu"])[:depth].reshape(depth, 2, 128)], axis=1)
    shared["s5_vec_fm"] = np.ascontiguousarray(vec.transpose(3, 0, 1, 2))
    shared["gdn_cw_fm"] = np.ascontiguousarray(f(inp["gdn_conv_w"])[:depth].reshape(depth, 3, 12, 128).transpose(3, 0, 2, 1))
    shared["gdn_gate"] = np.ascontiguousarray(np.concatenate(
        [f(inp["gdn_a_log"])[:depth].reshape(depth, 8), f(inp["gdn_dt_bias"])[:depth].reshape(depth, 8)], axis=1))
    shared["gdn_nw"] = f(inp["gdn_norm_w"])[:depth]
    shared["w_z"] = np.ascontiguousarray(w_in[:, :, 2560:3072])
    shared["w_g"] = np.ascontiguousarray(w_in[:, :, 3072:3088])
    shared["moe_wr"] = np.ascontiguousarray(np.concatenate([f(inp["moe_w_group"])[:depth], f(inp["moe_w_expert"])[:depth]], axis=2))
    shared["moe_rb"] = np.ascontiguousarray(np.concatenate([f(inp["moe_b_group"])[:depth], f(inp["moe_b_expert"])[:depth]], axis=1))
    shared["moe_wgu"] = f(inp["moe_w_gate_up"])[:depth]
    shared["moe_wd"] = f(inp["moe_w_down"])[:depth]
    maps = []
    xp, xs = f(inp["x_prompt"]), f(inp["x_sample"])
    cp, cs = f(inp["c_prompt"]), f(inp["c_sample"])
    for c in range(ncores):
        m = dict(shared)
        m["x_in"] = np.concatenate([xp[c, :LP], xs[c, :LS]], axis=0)
        cc = np.stack([cp[c], cs[c]], axis=-1)
        m["cT"] = np.ascontiguousarray(cc.reshape(KC, 128, 2).transpose(1, 0, 2))
        maps.append(m)
    return maps


_CACHE = {}


def run(inp, LP, LS, depth, ncores, flags=None):
    key = (LP, LS, depth, str(flags))
    nc, P = build(LP, LS, depth, flags)
    maps = make_in_maps(inp, LP, LS, depth, ncores)
    used = set()
    res = run_bass_kernel_spmd(nc, maps, core_ids=list(range(ncores)))
    yp = np.stack([res.results[c]["y_out"][:LP] for c in range(ncores)], axis=0)
    ys = np.stack([res.results[c]["y_out"][LP:LP + LS] for c in range(ncores)], axis=0)
    return yp.astype(np.float32), ys.astype(np.float32)


KFLAGS = {"s5": True, "conv": True, "gdn": True, "moe": True}


def kernel(**inputs):
    return run(inputs, 4096, 2048, 4, 8, KFLAGS)
```
